# Optimizing a Trainium2 kernel written in Bass

```python
import math
import jax, jax.numpy as jnp
from jax import lax
import numpy as np

D_MODEL = 1024
BATCH = 8
SEQ = 4096
DEPTH = 2

N_EVEN = (DEPTH + 1) // 2
N_ODD = DEPTH // 2

A_QK_DIM = 64
A_V_DIM = 2 * A_QK_DIM
A_WIDTH = D_MODEL // 2
A_HEADS = A_WIDTH // A_V_DIM
ROPE_DIM = A_QK_DIM // 4
ROPE_THETA = 500000.0
Q_BLOCK = 128

B_WIDTH = D_MODEL // 2
B_HEADS = 8
B_HEAD_DIM = B_WIDTH // B_HEADS
CONV_WIDTH = 4
LRU_C = 8.0

IN_WIDTH = 3 * A_WIDTH + 2 * B_WIDTH

POOL_WINDOWS = (2, 4, 8, 16)
POOL_GROUPS = len(POOL_WINDOWS)
POOL_GROUP_DIM = D_MODEL // POOL_GROUPS

FFN_DIM = 2816
N_EXPERTS = 8
TOP_K = 2
EXPERT_DIM = 3584

NORM_EPS = 1e-6
SUBLN_EPS = 1e-5

kernel_name = "hybrid_diffattn_rglru_pool_moe"


def rms_norm(x, g, eps=NORM_EPS):
    xf = x.astype(jnp.float32)
    y = xf * lax.rsqrt(jnp.mean(xf * xf, axis=-1, keepdims=True) + eps)
    return (y * g.astype(jnp.float32)).astype(x.dtype)


def rope_tables(seq):
    inv = ROPE_THETA ** (-jnp.arange(0, ROPE_DIM, 2, dtype=jnp.float32) / ROPE_DIM)
    ang = jnp.arange(seq, dtype=jnp.float32)[:, None] * inv[None, :]
    return jnp.cos(ang), jnp.sin(ang)


def apply_partial_rope(x, cos, sin):
    half = ROPE_DIM // 2
    x1 = x[..., :half].astype(jnp.float32)
    x2 = x[..., half:ROPE_DIM].astype(jnp.float32)
    c = cos[None, :, None, None, :]
    s = sin[None, :, None, None, :]
    rot = jnp.concatenate([x1 * c - x2 * s, x2 * c + x1 * s], axis=-1).astype(x.dtype)
    return jnp.concatenate([rot, x[..., ROPE_DIM:]], axis=-1)


def diff_attention(q, k, v, lam, lam_init, subln_g):
    bsz, seq = q.shape[0], q.shape[1]
    nb = seq // Q_BLOCK
    q = q * (A_QK_DIM ** -0.5)
    qb = q.reshape(bsz, nb, Q_BLOCK, A_HEADS, 2, A_QK_DIM).transpose(1, 0, 2, 3, 4, 5)
    key_pos = jnp.arange(seq)

    def block(args):
        q_blk, bi = args
        s = jnp.einsum("bqhmd,bkhmd->bhmqk", q_blk, k).astype(jnp.float32)
        q_pos = bi * Q_BLOCK + jnp.arange(Q_BLOCK)
        mask = key_pos[None, :] <= q_pos[:, None]
        p = jax.nn.softmax(jnp.where(mask, s, -jnp.inf), axis=-1)
        w = p[:, :, 0] - lam * p[:, :, 1]
        return jnp.einsum("bhqk,bkhd->bqhd", w.astype(v.dtype), v)

    o = lax.map(block, (qb, jnp.arange(nb)))
    o = o.transpose(1, 0, 2, 3, 4).reshape(bsz, seq, A_HEADS, A_V_DIM)
    o = rms_norm(o, subln_g, SUBLN_EPS) * (1.0 - lam_init)
    return o.reshape(bsz, seq, A_WIDTH)


def causal_depthwise_conv(x, w, b):
    y = lax.conv_general_dilated(
        x, w[:, None, :], window_strides=(1,), padding=[(CONV_WIDTH - 1, 0)],
        dimension_numbers=("NWC", "WIO", "NWC"), feature_group_count=x.shape[-1])
    return y + b


def rg_lru(x, wa, ba, wx, bx, lam):
    bsz, seq, ch = x.shape
    xh = x.reshape(bsz, seq, B_HEADS, B_HEAD_DIM)
    r = jax.nn.sigmoid((jnp.einsum("bshi,hij->bshj", xh, wa) + ba).astype(jnp.float32)).reshape(bsz, seq, ch)
    i = jax.nn.sigmoid((jnp.einsum("bshi,hij->bshj", xh, wx) + bx).astype(jnp.float32)).reshape(bsz, seq, ch)
    log_a = -LRU_C * r * jax.nn.softplus(-lam.astype(jnp.float32))
    a = jnp.exp(log_a)
    u = jnp.sqrt(-jnp.expm1(2.0 * log_a)) * (i * x.astype(jnp.float32))

    def combine(left, right):
        a1, b1 = left
        a2, b2 = right
        return a1 * a2, a2 * b1 + b2

    _, h = lax.associative_scan(combine, (a, u), axis=1)
    return h.astype(x.dtype)


def even_mixer(h, w_in, lq1, lk1, lq2, lk2, subln_g, conv_w, conv_b,
               wa, ba, wx, bx, lam_p, w_out, lam_init, cos, sin):
    bsz, seq, _ = h.shape
    proj = h @ w_in
    q, k, v, xb, gb = jnp.split(
        proj, [A_WIDTH, 2 * A_WIDTH, 3 * A_WIDTH, 3 * A_WIDTH + B_WIDTH], axis=-1)
    q = apply_partial_rope(q.reshape(bsz, seq, A_HEADS, 2, A_QK_DIM), cos, sin)
    k = apply_partial_rope(k.reshape(bsz, seq, A_HEADS, 2, A_QK_DIM), cos, sin)
    v = v.reshape(bsz, seq, A_HEADS, A_V_DIM)
    lam = (jnp.exp(jnp.sum(lq1.astype(jnp.float32) * lk1.astype(jnp.float32)))
           - jnp.exp(jnp.sum(lq2.astype(jnp.float32) * lk2.astype(jnp.float32)))
           + lam_init)
    attn = diff_attention(q, k, v, lam, lam_init, subln_g)
    rec = rg_lru(causal_depthwise_conv(xb, conv_w, conv_b), wa, ba, wx, bx, lam_p)
    rec = rec * jax.nn.gelu(gb)
    return jnp.concatenate([attn, rec], axis=-1) @ w_out


def pool_mixer(h, pool_w, pool_scale):
    bsz, seq, _ = h.shape
    hg = h.reshape(bsz, seq, POOL_GROUPS, POOL_GROUP_DIM)
    pos = jnp.arange(seq)
    outs = []
    for g, w in enumerate(POOL_WINDOWS):
        xg = hg[:, :, g].astype(jnp.float32)
        cs = jnp.cumsum(xg, axis=1)
        lower = jnp.pad(cs, ((0, 0), (w, 0), (0, 0)))[:, :seq]
        count = jnp.minimum(pos + 1, w).astype(jnp.float32)[None, :, None]
        outs.append((cs - lower) / count - xg)
    d = jnp.stack(outs, axis=2).astype(h.dtype)
    y = jnp.einsum("bsgc,gcd->bsgd", d, pool_w).reshape(bsz, seq, D_MODEL)
    return y * pool_scale


def swiglu(h, wg, wu, wd):
    return (jax.nn.silu(h @ wg) * (h @ wu)) @ wd


def moe_swiglu(h, router_w, wg, wu, wd):
    bsz, seq, dm = h.shape
    t = h.reshape(-1, dm)
    logits = (t @ router_w).astype(jnp.float32)
    top_val, top_idx = lax.top_k(logits, TOP_K)
    gates = jax.nn.softmax(top_val, axis=-1)
    combine = jnp.sum(jax.nn.one_hot(top_idx, N_EXPERTS, dtype=jnp.float32) * gates[..., None], axis=1)
    out = jnp.zeros_like(t)
    for e in range(N_EXPERTS):
        he = jax.nn.silu(t @ wg[e]) * (t @ wu[e])
        out = out + (combine[:, e:e + 1].astype(t.dtype) * he) @ wd[e]
    return out.reshape(bsz, seq, dm)


def setup_inputs(seed: int = 0) -> dict:
    key = jax.random.key(seed)
    ks = iter(jax.random.split(key, 40))
    f32 = jnp.float32

    def nrm(shape, scale):
        return jax.random.normal(next(ks), shape, f32) * scale

    def gain(shape):
        return 1.0 + 0.02 * jax.random.normal(next(ks), shape, f32)

    x = jax.random.normal(next(ks), (BATCH, SEQ, D_MODEL), f32)
    u = jax.random.uniform(next(ks), (N_EVEN, B_WIDTH), f32, 0.9, 0.999)
    s = u ** (1.0 / LRU_C)
    rg_lam = jnp.log(s / (1.0 - s))
    return {
        "x": x,
        "ln_mix_even": gain((N_EVEN, D_MODEL)),
        "w_in_even": nrm((N_EVEN, D_MODEL, IN_WIDTH), D_MODEL ** -0.5),
        "lam_q1": nrm((N_EVEN, A_QK_DIM), 0.1),
        "lam_k1": nrm((N_EVEN, A_QK_DIM), 0.1),
        "lam_q2": nrm((N_EVEN, A_QK_DIM), 0.1),
        "lam_k2": nrm((N_EVEN, A_QK_DIM), 0.1),
        "subln_g": gain((N_EVEN, A_V_DIM)),
        "conv_w": nrm((N_EVEN, CONV_WIDTH, B_WIDTH), CONV_WIDTH ** -0.5),
        "conv_b": nrm((N_EVEN, B_WIDTH), 0.01),
        "rg_wa": nrm((N_EVEN, B_HEADS, B_HEAD_DIM, B_HEAD_DIM), B_HEAD_DIM ** -0.5),
        "rg_ba": nrm((N_EVEN, B_HEADS, B_HEAD_DIM), 0.01),
        "rg_wx": nrm((N_EVEN, B_HEADS, B_HEAD_DIM, B_HEAD_DIM), B_HEAD_DIM ** -0.5),
        "rg_bx": nrm((N_EVEN, B_HEADS, B_HEAD_DIM), 0.01),
        "rg_lam": rg_lam,
        "w_out_even": nrm((N_EVEN, A_WIDTH + B_WIDTH, D_MODEL), (A_WIDTH + B_WIDTH) ** -0.5),
        "ln_ffn_even": gain((N_EVEN, D_MODEL)),
        "ffn_wg": nrm((N_EVEN, D_MODEL, FFN_DIM), D_MODEL ** -0.5),
        "ffn_wu": nrm((N_EVEN, D_MODEL, FFN_DIM), D_MODEL ** -0.5),
        "ffn_wd": nrm((N_EVEN, FFN_DIM, D_MODEL), FFN_DIM ** -0.5),
        "ln_mix_odd": gain((N_ODD, D_MODEL)),
        "pool_w": nrm((N_ODD, POOL_GROUPS, POOL_GROUP_DIM, POOL_GROUP_DIM), POOL_GROUP_DIM ** -0.5),
        "pool_scale": gain((N_ODD, D_MODEL)),
        "ln_ffn_odd": gain((N_ODD, D_MODEL)),
        "router_w": nrm((N_ODD, D_MODEL, N_EXPERTS), D_MODEL ** -0.5),
        "moe_wg": nrm((N_ODD, N_EXPERTS, D_MODEL, EXPERT_DIM), D_MODEL ** -0.5),
        "moe_wu": nrm((N_ODD, N_EXPERTS, D_MODEL, EXPERT_DIM), D_MODEL ** -0.5),
        "moe_wd": nrm((N_ODD, N_EXPERTS, EXPERT_DIM, D_MODEL), EXPERT_DIM ** -0.5),
        "final_g": gain((D_MODEL,)),
    }


def reference(x, ln_mix_even, w_in_even, lam_q1, lam_k1, lam_q2, lam_k2, subln_g,
              conv_w, conv_b, rg_wa, rg_ba, rg_wx, rg_bx, rg_lam, w_out_even,
              ln_ffn_even, ffn_wg, ffn_wu, ffn_wd, ln_mix_odd, pool_w, pool_scale,
              ln_ffn_odd, router_w, moe_wg, moe_wu, moe_wd, final_g):
    cos, sin = rope_tables(x.shape[1])
    h = x
    for layer in range(DEPTH):
        j = layer // 2
        if layer % 2 == 0:
            lam_init = 0.8 - 0.6 * math.exp(-0.3 * layer)
            h = h + even_mixer(rms_norm(h, ln_mix_even[j]), w_in_even[j],
                               lam_q1[j], lam_k1[j], lam_q2[j], lam_k2[j], subln_g[j],
                               conv_w[j], conv_b[j], rg_wa[j], rg_ba[j], rg_wx[j], rg_bx[j],
                               rg_lam[j], w_out_even[j], lam_init, cos, sin)
            h = h + swiglu(rms_norm(h, ln_ffn_even[j]), ffn_wg[j], ffn_wu[j], ffn_wd[j])
        else:
            h = h + pool_mixer(rms_norm(h, ln_mix_odd[j]), pool_w[j], pool_scale[j])
            h = h + moe_swiglu(rms_norm(h, ln_ffn_odd[j]), router_w[j],
                               moe_wg[j], moe_wu[j], moe_wd[j])
    return rms_norm(h, final_g)
```

```python
import contextlib
import math
import numpy as np
import concourse.bass as bass
import concourse.mybir as mybir
from concourse.bass_utils import run_bass_kernel_spmd

F32 = mybir.dt.float32
BF16 = mybir.dt.bfloat16
I32 = mybir.dt.int32
AF = mybir.ActivationFunctionType
ALU = mybir.AluOpType

ENGS = ("pe", "act", "dve", "pool", "sp")

D = 1024
KC = 8
IN_W = 2560
FFN = 2816
NE = 8
EXP = 3584
NORM_EPS = 1e-6
SUBLN_EPS = 1e-5
LAM_INIT0 = 0.8 - 0.6 * math.exp(-0.3 * 0)
TT = 512


class Buf:
    __slots__ = ("name", "last_write", "readers")

    def __init__(self, name=""):
        self.name = name
        self.last_write = None
        self.readers = {}


class Op:
    __slots__ = ("eng", "fn", "deps", "is_dma", "semkey", "ticket", "needed", "inc")

    def __init__(self, eng, fn, is_dma=False, semkey=None):
        self.eng = eng
        self.fn = fn
        self.deps = []
        self.is_dma = is_dma
        self.semkey = semkey if semkey is not None else eng
        self.ticket = None
        self.needed = False
        self.inc = 16 if is_dma else 1


class _Rec:
    def __init__(self):
        self.call = None

    def __getattr__(self, name):
        def f(*a, **k):
            self.call = (name, a, k)
            return None
        return f


def _bind(fn):
    rec = _Rec()
    fn(rec)
    name, a, k = rec.call
    return lambda e: getattr(e, name)(*a, **k)


class Prog:
    def __init__(self, nc, n_dma_sems=32):
        self.nc = nc
        self.ops = {e: [] for e in ENGS}
        self.all_ops = []
        self.last_op_per_sem = {}
        self.dma_cnt = {}

    def _deps(self, op, reads, writes):
        deps = []
        for b in reads:
            if b.last_write is not None:
                deps.append(b.last_write)
        for b in writes:
            if b.last_write is not None:
                deps.append(b.last_write)
            deps.extend(b.readers.values())
        out = []
        for d in deps:
            if d is op:
                continue
            if d.eng == op.eng and op.eng == "pe" and not d.is_dma and not op.is_dma:
                continue
            out.append(d)
        op.deps = out
        for d in out:
            d.needed = True
        for b in reads:
            b.readers[op.semkey] = op
        for b in writes:
            b.last_write = op
            b.readers = {}

    def op(self, eng, fn, reads=(), writes=()):
        o = Op(eng, _bind(fn))
        self._deps(o, reads, writes)
        self.ops[eng].append(o)
        self.all_ops.append(o)
        self.last_op_per_sem[o.semkey] = o
        return o

    def dma(self, eng, out_ap, in_ap, reads=(), writes=(), **kw):
        k = self._dma_key(eng)
        fn = lambda e: e.dma_start(out=out_ap, in_=in_ap, **kw)
        o = Op(eng, fn, is_dma=True, semkey=k)
        self._deps(o, reads, writes)
        prev = self.last_op_per_sem.get(k)
        if prev is not None:
            o.deps.append(prev)
            prev.needed = True
        self.ops[eng].append(o)
        self.all_ops.append(o)
        self.last_op_per_sem[k] = o
        return o

    def dma_custom(self, eng, fn, reads=(), writes=()):
        k = self._dma_key(eng)
        o = Op(eng, fn, is_dma=True, semkey=k)
        self._deps(o, reads, writes)
        prev = self.last_op_per_sem.get(k)
        if prev is not None:
            o.deps.append(prev)
            prev.needed = True
        self.ops[eng].append(o)
        self.all_ops.append(o)
        self.last_op_per_sem[k] = o
        return o

    def _dma_key(self, eng):
        n = {"sp": 24, "pool": 12}.get(eng, 4)
        c = self.dma_cnt.get(eng, 0)
        self.dma_cnt[eng] = c + 1
        return ("dma", eng, c % n)

    def barrier(self):
        lasts = list(self.last_op_per_sem.values())
        for e in ENGS:
            o = Op(e, None)
            o.deps = list(lasts)
            for d in lasts:
                d.needed = True
            self.ops[e].append(o)
            self.all_ops.append(o)

    def final_wait(self, eng="sp"):
        lasts = list(self.last_op_per_sem.values())
        o = Op(eng, None)
        o.deps = lasts
        for d in lasts:
            d.needed = True
        self.ops[eng].append(o)
        self.all_ops.append(o)

    def emit(self):
        nc = self.nc
        counts = {}
        for o in self.all_ops:
            if o.fn is None:
                continue
            if o.needed:
                counts[o.semkey] = counts.get(o.semkey, 0) + o.inc
                o.ticket = counts[o.semkey]
        self.max_counts = counts
        with contextlib.ExitStack() as st:
            sems = {}
            for k in counts:
                nm = k if isinstance(k, str) else f"dma_{k[1]}{k[2]}"
                sems[k] = st.enter_context(nc.semaphore(f"s_{nm}"))
            block = st.enter_context(nc.Block())
            engmap = {"pe": block.tensor, "act": block.scalar, "dve": block.vector,
                      "pool": block.gpsimd, "sp": block.sync}

            def make(ename):
                oplist = self.ops[ename]

                def body(e):
                    waited = {}
                    for o in oplist:
                        need = {}
                        for d in o.deps:
                            if d.ticket is None:
                                continue
                            if need.get(d.semkey, 0) < d.ticket:
                                need[d.semkey] = d.ticket
                        for k, t in need.items():
                            if waited.get(k, 0) < t:
                                e.wait_ge(sems[k], t)
                                waited[k] = t
                        if o.fn is None:
                            continue
                        ins = o.fn(e)
                        if o.needed:
                            ins.then_inc(sems[o.semkey], o.inc)
                return body

            for ename in ENGS:
                if self.ops[ename]:
                    engmap[ename](make(ename))


class Arena:
    def __init__(self, nc, name, nwords):
        self.t = nc.alloc_sbuf_tensor(name, [128, nwords], F32)
        self.n = nwords
        self.off = 0
        self.marks = []

    def mark(self):
        self.marks.append(self.off)

    def release(self):
        self.off = self.marks.pop()

    def f32(self, n):
        n = (n + 1) // 2 * 2
        assert self.off + n <= self.n, f"arena overflow {self.off}+{n}>{self.n}"
        ap = self.t[:, self.off:self.off + n]
        self.off += n
        return ap

    def bf16(self, n):
        w = (n + 3) // 4 * 2
        return self.f32(w).bitcast(BF16)[:, 0:n]


def build(S, dbg=False, stop_after=None, n_exp=NE):
    NT = S // TT
    NS = S // 128
    nc = bass.Bass("TRN2", target_bir_lowering=False)

    def din(name, shape, dt=F32):
        return nc.dram_tensor(name, list(shape), dt, kind="ExternalInput").ap()

    x = din("x", [S, D])
    ln_mix_even = din("ln_mix_even", [D]); ln_ffn_even = din("ln_ffn_even", [D])
    ln_mix_odd = din("ln_mix_odd", [D]); ln_ffn_odd = din("ln_ffn_odd", [D])
    final_g = din("final_g", [D]); pool_scale = din("pool_scale", [D])
    w_in = din("w_in_even", [D, IN_W]); w_out = din("w_out_even", [D, D])
    lam_q1 = din("lam_q1", [64]); lam_k1 = din("lam_k1", [64])
    lam_q2 = din("lam_q2", [64]); lam_k2 = din("lam_k2", [64])
    subln_g = din("subln_g", [128])
    conv_w = din("conv_w", [4, 512]); conv_b = din("conv_b", [512])
    rg_wa = din("rg_wa", [8, 64, 64]); rg_ba = din("rg_ba", [512])
    rg_wx = din("rg_wx", [8, 64, 64]); rg_bx = din("rg_bx", [512])
    rg_lam = din("rg_lam", [512])
    ffn_wg = din("ffn_wg", [D, FFN]); ffn_wu = din("ffn_wu", [D, FFN]); ffn_wd = din("ffn_wd", [FFN, D])
    pool_w = din("pool_w", [4, 256, 256])
    router_w = din("router_w", [D, NE])
    moe_wg = din("moe_wg", [NE, D, EXP]); moe_wu = din("moe_wu", [NE, D, EXP]); moe_wd = din("moe_wd", [NE, EXP, D])
    ropet = din("ropet", [4, 128, S])
    invcnt = din("invcnt", [128, 8, 16])
    y = nc.dram_tensor("y", [S, D], F32, kind="ExternalOutput").ap()
    h1d = nc.dram_tensor("h1d", [S, D], F32, kind="ExternalOutput" if dbg else "Internal").ap()

    P = Prog(nc)
    A = Arena(nc, "arena", 52224)
    psum = [nc.alloc_psum_tensor(f"ps{i}", [128, 512], F32)[:, :] for i in range(8)]
    pbuf = [Buf(f"ps{i}") for i in range(8)]
    ybuf = Buf("y")
    h1buf = [Buf(f"h1_{i}") for i in range(NS)]

    def dump(name, ap, bufs, dt=BF16):
        if not dbg:
            return
        t = nc.dram_tensor(name, list(ap.shape), dt, kind="ExternalOutput").ap()
        P.dma("sp", t, ap, reads=list(bufs), writes=[Buf()])

    ident = A.bf16(128); b_ident = Buf()
    identf = A.f32(128); b_identf = Buf()
    ones_bf = A.bf16(128); b_ones = Buf()
    cst = A.f32(8); b_cst = Buf()
    P.op("pool", lambda e: e.memset(ident, 1.0), writes=[b_ident])
    P.op("pool", lambda e: e.affine_select(out=ident, in_=ident, pattern=[[-1, 128]], compare_op=ALU.is_equal,
                                           fill=0.0, base=0, channel_multiplier=1), reads=[b_ident], writes=[b_ident])
    P.op("pool", lambda e: e.memset(identf, 1.0), writes=[b_identf])
    P.op("pool", lambda e: e.affine_select(out=identf, in_=identf, pattern=[[-1, 128]], compare_op=ALU.is_equal,
                                           fill=0.0, base=0, channel_multiplier=1), reads=[b_identf], writes=[b_identf])
    P.op("pool", lambda e: e.memset(ones_bf, 1.0), writes=[b_ones])
    tri01 = A.bf16(128); b_tri = Buf()
    P.op("pool", lambda e: e.memset(tri01, 1.0), writes=[b_tri])
    P.op("pool", lambda e: e.affine_select(out=tri01, in_=tri01, pattern=[[1, 128]], compare_op=ALU.is_ge, fill=0.0, base=0, channel_multiplier=-1),
         reads=[b_tri], writes=[b_tri])
    P.op("pool", lambda e: e.memset(cst[:, 0:1], -0.5), writes=[b_cst])
    P.op("pool", lambda e: e.memset(cst[:, 1:2], NORM_EPS), writes=[b_cst])
    P.op("pool", lambda e: e.memset(cst[:, 2:3], SUBLN_EPS), writes=[b_cst])
    P.op("pool", lambda e: e.memset(cst[:, 3:4], 1.0), writes=[b_cst])

    def load_bcast(vec, n, name):
        t = A.f32(n); b = Buf(name)
        P.dma("sp", t, vec.partition_broadcast(128), writes=[b])
        return t, b

    def dscr(name, shape, dt=BF16):
        return nc.dram_tensor(name, list(shape), dt, kind="Internal").ap()

    w_in_b = dscr("w_in_b", [D, IN_W]); w_out_b = dscr("w_out_b", [D, D]); pool_w_b = dscr("pool_w_b", [4, 256, 256])
    NFG = FFN // 256
    ffn_gq = dscr("ffn_gq", [NFG, 128, KC * 256]); ffn_uq = dscr("ffn_uq", [NFG, 128, KC * 256]); ffn_dq = dscr("ffn_dq", [NFG, 128, 2 * D])
    QWc = EXP // 4
    moe_gq = [dscr(f"moe_gq{q}", [NE * 128, KC * QWc]) for q in range(4)]
    moe_uq = [dscr(f"moe_uq{q}", [NE * 128, KC * QWc]) for q in range(4)]
    moe_dq = [dscr(f"moe_dq{q}", [NE * 128, 7 * D]) for q in range(4)]
    xnTd = dscr("xnTd", [NT, 128, KC * TT]); b_xnTd = [Buf() for _ in range(NT)]
    TS = 384
    NSLOT_T = (2 * S + NE * (TS - 1)) // TS
    NSLOT = NSLOT_T * TS
    Xg = dscr("Xg", [NSLOT, D]); b_Xg = Buf()
    pc_q = []
    b_winb = [Buf() for _ in range(KC)]; b_woutb = [Buf() for _ in range(KC)]; b_pwb = [Buf() for _ in range(4)]
    b_fg = [Buf() for _ in range(FFN // 256)]; b_fu = [Buf() for _ in range(FFN // 256)]; b_fd = [Buf() for _ in range(FFN // 256)]
    b_moe = []

    def pc(dst, src, buf, now=False):
        if now:
            P.dma("pool", dst, src, writes=[buf])
        else:
            pc_q.append((dst, src, buf))

    def tick(n=1):
        for _ in range(n):
            if pc_q:
                dst, src, buf = pc_q.pop(0)
                P.dma("pool", dst, src, writes=[buf])

    for kc in range(KC):
        pc(w_in_b[kc * 128:(kc + 1) * 128, :], w_in[kc * 128:(kc + 1) * 128, :], b_winb[kc], now=True)
    for kc in range(KC):
        pc(w_out_b[kc * 128:(kc + 1) * 128, :], w_out[kc * 128:(kc + 1) * 128, :], b_woutb[kc])
    for g in range(4):
        pc(pool_w_b[g], pool_w[g], b_pwb[g])
    for g in range(FFN // 256):
        pc(ffn_gq[g].rearrange("p (k n) -> p k n", k=KC), ffn_wg.rearrange("(k p) n -> p k n", p=128)[:, :, g * 256:(g + 1) * 256], b_fg[g])
        pc(ffn_uq[g].rearrange("p (k n) -> p k n", k=KC), ffn_wu.rearrange("(k p) n -> p k n", p=128)[:, :, g * 256:(g + 1) * 256], b_fu[g])
        pc(ffn_dq[g].rearrange("p (c n) -> p c n", c=2), ffn_wd[g * 256:(g + 1) * 256, :].rearrange("(c p) n -> p c n", p=128), b_fd[g])
    for ex in range(NE):
        for q in range(4):
            for (dst, src) in ((moe_gq, moe_wg), (moe_uq, moe_wu)):
                b = Buf(); b_moe.append(b)
                pc(dst[q][ex * 128:(ex + 1) * 128, :].rearrange("p (k n) -> p k n", k=KC),
                   src[ex].rearrange("(k p) n -> p k n", p=128)[:, :, q * QWc:(q + 1) * QWc], b)
            b = Buf(); b_moe.append(b)
            pc(moe_dq[q][ex * 128:(ex + 1) * 128, :].rearrange("p (c n) -> p c n", c=7),
               moe_wd[ex, q * 7 * 128:(q + 1) * 7 * 128, :].rearrange("(c p) n -> p c n", p=128), b)

    def norm_T(src, b_src, g_bc, b_g, dstT, b_dstT, col0, tmp, ps_i, f32T=None, split=False):
        if isinstance(tmp, list):
            tmp = tmp[nrr[0] % len(tmp)]; nrr[0] += 1
        junk, ss, rstd, xn, b_tmp = tmp
        P.op("act", lambda e: e.activation(out=junk, in_=src, func=AF.Square, accum_out=ss), reads=[b_src], writes=[b_tmp[0]])
        P.op("dve", lambda e: e.tensor_scalar(out=rstd, in0=ss, scalar1=1.0 / D, scalar2=NORM_EPS, op0=ALU.mult, op1=ALU.add),
             reads=[b_tmp[0]], writes=[b_tmp[1]])
        P.op("pool", lambda e: e.tensor_tensor(out=rstd, in0=rstd, in1=cst[:, 0:1], op=ALU.pow), reads=[b_tmp[1], b_cst], writes=[b_tmp[1]])
        tick()
        if f32T is None:
            P.op("dve", lambda e: e.scalar_tensor_tensor(out=xn, in0=src, scalar=rstd, in1=g_bc, op0=ALU.mult, op1=ALU.mult),
                 reads=[b_src, b_tmp[1], b_g], writes=[b_tmp[2]])
        else:
            xT32, b_xT32, xn32, b_xn32, psi32 = f32T
            P.op("dve", lambda e: e.scalar_tensor_tensor(out=xn32, in0=src, scalar=rstd, in1=g_bc, op0=ALU.mult, op1=ALU.mult),
                 reads=[b_src, b_tmp[1], b_g], writes=[b_xn32])
            P.op("act", lambda e: e.activation(out=xn, in_=xn32, func=AF.Copy), reads=[b_xn32], writes=[b_tmp[2]])
            for hh in range(2):
                pp = psum[psi32[hh]]
                for c4 in range(4):
                    c = hh * 4 + c4
                    P.op("pe", lambda e, c=c, c4=c4, pp=pp: e.transpose(out=pp[:, c4 * 128:(c4 + 1) * 128], in_=xn32[:, c * 128:(c + 1) * 128], identity=identf),
                         reads=[b_xn32, b_identf], writes=[pbuf[psi32[hh]]])
                P.op("dve", lambda e, hh=hh, pp=pp: e.tensor_copy(out=xT32[:, hh * 4:(hh + 1) * 4, :], in_=pp.rearrange("p (c t) -> p c t", c=4)),
                     reads=[pbuf[psi32[hh]]], writes=[b_xT32])
        def part_b():
            pb = psum[ps_i].bitcast(BF16)
            for c in range(KC):
                P.op("pe", lambda e, c=c: e.transpose(out=pb[:, c * 128:(c + 1) * 128], in_=xn[:, c * 128:(c + 1) * 128], identity=ident),
                     reads=[b_tmp[2], b_ident], writes=[pbuf[ps_i]])
            P.op("act", lambda e: e.activation(out=dstT[:, :, col0:col0 + 128], in_=pb.rearrange("p (c t) -> p c t", c=KC), func=AF.Copy),
                 reads=[pbuf[ps_i]], writes=[b_dstT])
        if split:
            return part_b
        part_b()

    nrr = [0]

    def norm_tmp1():
        junk = A.bf16(D); ss = A.f32(2); rstd = A.f32(2); xn = A.bf16(D)
        return (junk, ss[:, 0:1], rstd[:, 0:1], xn, [Buf(), Buf(), Buf()])

    def norm_tmp(n=1):
        if n == 1:
            return norm_tmp1()
        sets = [norm_tmp1()]
        for _ in range(n - 1):
            ss = A.f32(2); rstd = A.f32(2); xn = A.bf16(D)
            sets.append((sets[0][0], ss[:, 0:1], rstd[:, 0:1], xn, [sets[0][4][0], Buf(), Buf()]))
        return sets

    A.mark()
    g0_bc, b_g0 = load_bcast(ln_mix_even, D, "g0")
    qT = A.bf16(4 * S).rearrange("p (c t) -> p c t", c=4); b_qT = [Buf() for _ in range(NT)]
    attnT = qT; b_attnT = b_qT
    ps_rr = [0]

    def next_ps(lo, hi):
        i = lo + ps_rr[0] % (hi - lo)
        ps_rr[0] += 1
        return i

    def in_proj_fm(wt, b_wt, col0, ps_i, xnT, b_xnT, kcn=KC):
        pp = psum[ps_i]
        for kc in range(kcn):
            P.op("pe", lambda e, kc=kc: e.matmul(pp, lhsT=wt[:, kc, col0:col0 + 128], rhs=xnT[:, kc, :], start=(kc == 0), stop=(kc == kcn - 1)),
                 reads=[b_wt, b_xnT], writes=[pbuf[ps_i]])

    tick(12)
    A.mark()
    kT = A.bf16(4 * S).rearrange("p (c t) -> p c t", c=4); b_kT = [Buf() for _ in range(NT)]
    Vt = A.bf16(NS * 512).rearrange("p (s n) -> p s n", s=NS); b_V = [Buf() for _ in range(NT)]
    A.mark()
    w_qkv = A.bf16(KC * 1536).rearrange("p (k n) -> p k n", k=KC); b_wqkv = Buf()
    for kc in range(KC):
        P.dma("sp", w_qkv[:, kc, :], w_in_b[kc * 128:(kc + 1) * 128, 0:1536], reads=[b_winb[kc]], writes=[b_wqkv])
    wsw = A.bf16(KC * 1024).rearrange("p (k n) -> p k n", k=KC); b_wsw = Buf()
    P.op("pool", lambda e: e.memset(wsw.rearrange("p k n -> p (k n)"), 0.0), writes=[b_wsw])
    srcv = w_qkv[:, :, 0:1024].rearrange("p k (g d) -> p k g d", d=64)
    dstv = wsw.rearrange("p k (g d) -> p k g d", d=64)
    P.op("dve", lambda e: e.tensor_copy(out=dstv[:, :, :, 0:8], in_=srcv[:, :, :, 8:16]), reads=[b_wqkv, b_wsw], writes=[b_wsw])
    P.op("dve", lambda e: e.tensor_copy(out=dstv[:, :, :, 8:16], in_=srcv[:, :, :, 0:8]), reads=[b_wqkv, b_wsw], writes=[b_wsw])
    zt = A.bf16(D); b_zt = Buf()
    P.op("dve", lambda e: e.memset(zt, 0.0), writes=[b_zt])
    zrows = list(range(0, NSLOT, 128))
    xt = [A.f32(D) for _ in range(2)]; b_xt = [Buf(), Buf()]
    ntmp = norm_tmp(2)
    xnTq = [A.bf16(KC * TT).rearrange("p (c t) -> p c t", c=KC) for _ in range(2)]; b_xnTq = [Buf(), Buf()]
    rtab = [A.f32(4 * TT).rearrange("p (f t) -> p f t", f=4) for _ in range(2)]; b_rtab = [Buf(), Buf()]
    t1 = [A.f32(TT) for _ in range(2)]; t2 = [A.f32(TT) for _ in range(2)]
    b_t1 = [Buf(), Buf()]; b_t2 = [Buf(), Buf()]

    def prep_q(tt):
        pend = []
        for s in range(4):
            tok0 = tt * TT + s * 128
            xi = (tt * 4 + s) % 2
            P.dma("sp", xt[xi], x[tok0:tok0 + 128, :], writes=[b_xt[xi]])
            pend.append(norm_T(xt[xi], b_xt[xi], g0_bc, b_g0, xnTq[tt % 2], b_xnTq[tt % 2], s * 128, ntmp, 7, split=True))
            yield
            if len(pend) > 1:
                pend.pop(0)()
            yield
        while pend:
            yield
            pend.pop(0)()
        P.dma("sp", xnTd[tt], xnTq[tt % 2].rearrange("p c t -> p (c t)"), reads=[b_xnTq[tt % 2]], writes=[b_xnTd[tt]])
        yield

    for _ in prep_q(0):
        pass
    for tt in range(NT):
        ri = tt % 2
        xnT = xnTq[tt % 2]; b_xnT = b_xnTq[tt % 2]
        genq = [prep_q(tt + 1) if tt + 1 < NT else None]

        def advq(n, genq=genq):
            for _ in range(n):
                if genq[0] is None:
                    return
                try:
                    next(genq[0])
                except StopIteration:
                    genq[0] = None

        P.dma("sp", rtab[ri], ropet[:, :, tt * TT:(tt + 1) * TT].rearrange("f p t -> p f t"), writes=[b_rtab[ri]])
        for _ in range((len(zrows) + NT - 1 - tt) // (NT - tt) if tt < NT - 1 else len(zrows)):
            if zrows:
                r0 = zrows.pop(0)
                P.dma("sp", Xg[r0:r0 + 128, :], zt, reads=[b_zt], writes=[Buf()])
        for hc in range(8):
            p1 = next_ps(0, 6);
            in_proj_fm(w_qkv, b_wqkv, hc * 128, p1, xnT, b_xnT)
            p2 = next_ps(0, 6)
            in_proj_fm(wsw, b_wsw, hc * 128, p2, xnT, b_xnT)
            isk = hc >= 4
            q = hc % 2
            P.op("dve", lambda e, q=q, p1=p1, ri=ri, isk=isk: e.tensor_tensor(out=t1[q], in0=psum[p1], in1=rtab[ri][:, 2 if isk else 0, :], op=ALU.mult),
                 reads=[pbuf[p1], b_rtab[ri]], writes=[b_t1[q]])
            P.op("dve", lambda e, q=q, p2=p2, ri=ri, isk=isk: e.tensor_tensor(out=t2[q], in0=psum[p2], in1=rtab[ri][:, 3 if isk else 1, :], op=ALU.mult),
                 reads=[pbuf[p2], b_rtab[ri]], writes=[b_t2[q]])
            dst = (kT if isk else qT)[:, hc % 4, tt * TT:(tt + 1) * TT]
            bd = (b_kT if isk else b_qT)[tt]
            P.op("pool", lambda e, q=q, dst=dst: e.tensor_tensor(out=dst, in0=t1[q], in1=t2[q], op=ALU.add), reads=[b_t1[q], b_t2[q]], writes=[bd])
            advq(1)
        for s in range(4):
            pv = next_ps(0, 6)
            for kc in range(KC):
                P.op("pe", lambda e, kc=kc, s=s, pv=pv: e.matmul(psum[pv], lhsT=xnT[:, kc, s * 128:(s + 1) * 128], rhs=w_qkv[:, kc, 1024:1536], start=(kc == 0), stop=(kc == KC - 1)),
                     reads=[b_xnT, b_wqkv], writes=[pbuf[pv]])
            P.op("act", lambda e, s=s, pv=pv, tt=tt: e.activation(out=Vt[:, tt * 4 + s, :], in_=psum[pv], func=AF.Copy), reads=[pbuf[pv]], writes=[b_V[tt]])
            advq(1)
        advq(10 ** 6)
    P.barrier()
    A.release()

    dump("d_qT", qT, b_qT); dump("d_kT", kT, b_kT); dump("d_V", Vt, b_V)
    P.barrier()
    A.mark()
    lv = [load_bcast(v_, 64, "lv")[0:2] for v_ in (lam_q1, lam_k1, lam_q2, lam_k2)]
    lsum = A.f32(4); b_ls = Buf(); ljunk = A.f32(64)
    neglam = A.f32(2); gsc = A.f32(2); b_nl = Buf()
    sg_t, b_sg = A.f32(2), Buf()
    P.dma("sp", sg_t[:, 0:1], subln_g.rearrange("(p o) -> p o", o=1), writes=[b_sg])
    P.op("dve", lambda e: e.tensor_tensor(out=ljunk, in0=lv[0][0], in1=lv[1][0], op=ALU.mult), reads=[lv[0][1], lv[1][1]], writes=[b_ls])
    P.op("dve", lambda e: e.tensor_reduce(out=lsum[:, 0:1], in_=ljunk, axis=mybir.AxisListType.X, op=ALU.add), reads=[b_ls], writes=[b_ls])
    P.op("dve", lambda e: e.tensor_tensor(out=ljunk, in0=lv[2][0], in1=lv[3][0], op=ALU.mult), reads=[lv[2][1], lv[3][1], b_ls], writes=[b_ls])
    P.op("dve", lambda e: e.tensor_reduce(out=lsum[:, 1:2], in_=ljunk, axis=mybir.AxisListType.X, op=ALU.add), reads=[b_ls], writes=[b_ls])
    P.op("act", lambda e: e.activation(out=lsum[:, 0:2], in_=lsum[:, 0:2], func=AF.Exp), reads=[b_ls], writes=[b_ls])
    P.op("dve", lambda e: e.tensor_tensor(out=neglam[:, 0:1], in0=lsum[:, 1:2], in1=lsum[:, 0:1], op=ALU.subtract), reads=[b_ls], writes=[b_nl])
    P.op("dve", lambda e: e.tensor_scalar(out=neglam[:, 0:1], in0=neglam[:, 0:1], scalar1=-LAM_INIT0, scalar2=None, op0=ALU.add), reads=[b_nl], writes=[b_nl])
    P.op("dve", lambda e: e.tensor_scalar(out=gsc[:, 0:1], in0=sg_t[:, 0:1], scalar1=(1.0 - LAM_INIT0), scalar2=None, op0=ALU.mult), reads=[b_sg], writes=[b_nl])
    ET = [[A.bf16(TT) for _ in range(2)] for _ in range(2)]; b_ET = [[Buf() for _ in range(2)] for _ in range(2)]
    r1 = A.f32(TT); r2 = A.f32(TT); o1 = A.f32(TT); osq = A.bf16(TT); b_ep = [Buf() for _ in range(4)]
    pend_epi = [None]
    for h in range(4):
        for qb in range(NT):
            q0 = qb * TT
            nkb = 4 * qb + 4
            def emit_S(kb):
                off = max(0, (kb - 4 * qb) * 128)
                n = TT - off
                eb = kb % 2
                for m in range(2):
                    ps_s = m * 2 + eb
                    P.op("pe", lambda e, m=m, kb=kb, off=off, ps_s=ps_s, n=n: e.matmul(
                        psum[ps_s][:, 0:n], lhsT=kT[m * 64:(m + 1) * 64, h, kb * 128:(kb + 1) * 128],
                        rhs=qT[m * 64:(m + 1) * 64, h, q0 + off:q0 + TT], start=True, stop=True),
                        reads=[b_kT[kb // 4], b_qT[qb]], writes=[pbuf[ps_s]])
                    P.op("act", lambda e, m=m, eb=eb, ps_s=ps_s, n=n: e.activation(out=ET[m][eb][:, 0:n], in_=psum[ps_s][:, 0:n], func=AF.Exp),
                         reads=[pbuf[ps_s]], writes=[b_ET[m][eb]])
                    if kb >= 4 * qb:
                        P.op("dve", lambda e, m=m, eb=eb: e.tensor_tensor(out=ET[m][eb][:, 0:128], in0=ET[m][eb][:, 0:128], in1=tri01, op=ALU.mult),
                             reads=[b_ET[m][eb], b_tri], writes=[b_ET[m][eb]])

            def emit_PV(kb):
                off = max(0, (kb - 4 * qb) * 128)
                n = TT - off
                eb = kb % 2
                for m in range(2):
                    P.op("pe", lambda e, m=m, eb=eb, kb=kb, off=off, n=n: e.matmul(
                        psum[4 + m][:, off:TT], lhsT=Vt[:, kb, h * 128:(h + 1) * 128], rhs=ET[m][eb][:, 0:n], start=(kb == 0), stop=(kb == nkb - 1)),
                        reads=[b_V[kb // 4], b_ET[m][eb]], writes=[pbuf[4 + m]])
                    P.op("pe", lambda e, m=m, eb=eb, kb=kb, off=off, n=n: e.matmul(
                        psum[6 + m][:, off:TT], lhsT=ones_bf, rhs=ET[m][eb][:, 0:n], start=(kb == 0), stop=(kb == nkb - 1)),
                        reads=[b_ones, b_ET[m][eb]], writes=[pbuf[6 + m]])

            emit_S(0)
            for kb in range(nkb):
                if kb + 1 < nkb:
                    emit_S(kb + 1)
                emit_PV(kb)
                if kb == 1 and pend_epi[0] is not None:
                    pend_epi[0]()
                    pend_epi[0] = None
            tick(3)
            P.op("dve", lambda e: e.reciprocal(out=r1, in_=psum[6]), reads=[pbuf[6]], writes=[b_ep[0]])
            P.op("dve", lambda e: e.reciprocal(out=r2, in_=psum[7]), reads=[pbuf[7]], writes=[b_ep[1]])
            P.op("dve", lambda e: e.tensor_tensor(out=r1, in0=psum[4], in1=r1, op=ALU.mult), reads=[pbuf[4], b_ep[0]], writes=[b_ep[0]])
            P.op("dve", lambda e: e.tensor_tensor(out=r2, in0=psum[5], in1=r2, op=ALU.mult), reads=[pbuf[5], b_ep[1]], writes=[b_ep[1]])
            P.op("dve", lambda e: e.scalar_tensor_tensor(out=o1, in0=r2, scalar=neglam[:, 0:1], in1=r1, op0=ALU.mult, op1=ALU.add),
                 reads=[b_ep[0], b_ep[1], b_nl], writes=[b_ep[2]])
            def epi_tail(h=h, q0=q0, qb=qb):
                P.op("act", lambda e: e.activation(out=osq, in_=o1, func=AF.Square), reads=[b_ep[2]], writes=[b_ep[3]])
                P.op("pe", lambda e: e.matmul(psum[1], lhsT=ones_bf, rhs=osq, start=True, stop=True), reads=[b_ones, b_ep[3]], writes=[pbuf[1]])
                P.op("act", lambda e: e.activation(out=r1, in_=psum[1], func=AF.Ln, scale=1.0 / 128, bias=cst[:, 2:3]), reads=[pbuf[1], b_cst, b_ep[0]], writes=[b_ep[0]])
                P.op("act", lambda e: e.activation(out=r1, in_=r1, func=AF.Exp, scale=-0.5), reads=[b_ep[0]], writes=[b_ep[0]])
                P.op("dve", lambda e: e.scalar_tensor_tensor(out=attnT[:, h, q0:q0 + TT], in0=o1, scalar=gsc[:, 0:1], in1=r1, op0=ALU.mult, op1=ALU.mult),
                     reads=[b_ep[2], b_ep[0], b_nl], writes=[b_attnT[qb]])
            pend_epi[0] = epi_tail
    pend_epi[0]()
    P.barrier()
    A.release()
    A.release()

    recT = A.bf16(4 * S).rearrange("p (c t) -> p c t", c=4); b_recT = [Buf() for _ in range(NT)]
    A.mark()
    w_rec = A.bf16(KC * 1024).rearrange("p (k n) -> p k n", k=KC); b_wrec = Buf()
    for kc in range(KC):
        P.dma("sp", w_rec[:, kc, :], w_in_b[kc * 128:(kc + 1) * 128, 1536:2560], reads=[b_winb[kc]], writes=[b_wrec])
    def load_cm(vec, name):
        t = A.f32(4); b = Buf(name)
        P.dma("sp", t, vec.rearrange("(c p) -> p c", p=128), writes=[b], allow_slow_non_contiguous=True)
        return t, b
    cw = A.f32(16).rearrange("p (j c) -> p j c", j=4); b_cw = Buf()
    P.dma("sp", cw, conv_w.rearrange("j (c p) -> p j c", p=128), writes=[b_cw], allow_slow_non_contiguous=True)
    cb, b_cb = load_cm(conv_b, "cb")
    ba_t, b_ba = load_cm(rg_ba, "ba")
    bx_t, b_bx = load_cm(rg_bx, "bx")
    lam_t, b_lam = load_cm(rg_lam, "lam")
    cneg = A.f32(4); c2 = A.f32(4); b_cn = Buf()
    P.op("act", lambda e: e.activation(out=cneg, in_=lam_t, func=AF.Exp, scale=-1.0), reads=[b_lam], writes=[b_cn])
    P.op("act", lambda e: e.activation(out=cneg, in_=cneg, func=AF.Ln, bias=cst[:, 3:4]), reads=[b_cn, b_cst], writes=[b_cn])
    P.op("dve", lambda e: e.tensor_scalar(out=c2, in0=cneg, scalar1=-16.0, scalar2=None, op0=ALU.mult), reads=[b_cn], writes=[b_cn])
    P.op("dve", lambda e: e.tensor_scalar(out=cneg, in0=cneg, scalar1=-8.0, scalar2=None, op0=ALU.mult), reads=[b_cn], writes=[b_cn])
    wbd_f = A.f32(2 * 4 * 128).rearrange("p (w c n) -> p w c n", w=2, c=4); b_wbdf = Buf()
    wbd = A.bf16(2 * 4 * 128).rearrange("p (w c n) -> p w c n", w=2, c=4); b_wbd = Buf()
    P.op("pool", lambda e: e.memset(wbd_f.rearrange("p w c n -> p (w c n)"), 0.0), writes=[b_wbdf])
    for wi, wsrc in enumerate((rg_wa, rg_wx)):
        for hl in range(2):
            P.dma("sp", wbd_f[hl * 64:(hl + 1) * 64, wi, :, hl * 64:(hl + 1) * 64],
                  wsrc.rearrange("(c h) i j -> h i c j", h=2)[hl], reads=[], writes=[b_wbdf])
    P.op("dve", lambda e: e.tensor_copy(out=wbd.rearrange("p w c n -> p (w c n)"), in_=wbd_f.rearrange("p w c n -> p (w c n)")),
         reads=[b_wbdf], writes=[b_wbd])

    xnT_r = [A.bf16(KC * TT).rearrange("p (c t) -> p c t", c=KC) for _ in range(2)]; b_xnT_r = [Buf(), Buf()]
    xbT = [A.f32(4 * (TT + 4)).rearrange("p (c t) -> p c t", c=4) for _ in range(2)]; b_xb = [Buf() for _ in range(2)]
    gbT = A.f32(4 * TT).rearrange("p (c t) -> p c t", c=4); b_gb = Buf()
    NTMP = 6
    rt = [[A.f32(TT) for _ in range(NTMP)] for _ in range(4)]
    b_rt = [[Buf() for _ in range(NTMP)] for _ in range(4)]
    xcb = [A.bf16(TT) for _ in range(4)]; b_xcb = [Buf() for _ in range(4)]
    hst = [A.f32(4 * TT).rearrange("p (c t) -> p c t", c=4) for _ in range(2)]; b_hst = [Buf() for _ in range(2)]
    P.op("pool", lambda e: e.memset(xbT[0][:, :, 0:4], 0.0), writes=[b_xb[0]])

    for tt in range(NT):
        cur = tt % 2; prv = 1 - cur
        xnT = xnT_r[cur]; b_xnT = b_xnT_r[cur]
        P.dma("sp", xnT.rearrange("p c t -> p (c t)"), xnTd[tt], reads=[b_xnTd[tt]], writes=[b_xnT])
        tick(4)
        for c in range(4):
            pi = next_ps(0, 4)
            in_proj_fm(w_rec, b_wrec, c * 128, pi, xnT, b_xnT)
            P.op("act", lambda e, c=c, pi=pi: e.activation(out=xbT[cur][:, c, 4:4 + TT], in_=psum[pi], func=AF.Copy), reads=[pbuf[pi]], writes=[b_xb[cur]])
        for c in range(4):
            pi = next_ps(0, 4)
            in_proj_fm(w_rec, b_wrec, 512 + c * 128, pi, xnT, b_xnT)
            P.op("act", lambda e, c=c, pi=pi: e.activation(out=gbT[:, c, :], in_=psum[pi], func=AF.Copy), reads=[pbuf[pi]], writes=[b_gb])
        if tt > 0:
            P.op("pool", lambda e, cur=cur, prv=prv: e.tensor_copy(out=xbT[cur][:, :, 1:4], in_=xbT[prv][:, :, TT + 1:TT + 4]),
                 reads=[b_xb[prv]], writes=[b_xb[cur]])
        XB = [xbT[cur][:, c, :] for c in range(4)]
        for c in range(4):
            xc = rt[c][0]; BT = b_rt[c]
            P.op("dve", lambda e, c=c, xc=xc: e.tensor_scalar(out=xc, in0=XB[c][:, 4:4 + TT], scalar1=cw[:, 3, c:c + 1], scalar2=cb[:, c:c + 1], op0=ALU.mult, op1=ALU.add),
                 reads=[b_xb[cur], b_cw, b_cb], writes=[BT[0]])
            for j in range(3):
                P.op("dve", lambda e, c=c, j=j, xc=xc: e.scalar_tensor_tensor(out=xc, in0=XB[c][:, 1 + j:1 + j + TT], scalar=cw[:, j, c:c + 1], in1=xc, op0=ALU.mult, op1=ALU.add),
                     reads=[b_xb[cur], b_cw, BT[0]], writes=[BT[0]])
            P.op("pool", lambda e, c=c, xc=xc: e.tensor_copy(out=xcb[c], in_=xc), reads=[BT[0]], writes=[b_xcb[c]])
        for c in range(4):
            gb_c = gbT[:, c, :]; sq = rt[c][5]; BT = b_rt[c]
            P.op("pool", lambda e, gb_c=gb_c, sq=sq: e.tensor_tensor(out=sq, in0=gb_c, in1=gb_c, op=ALU.mult), reads=[b_gb], writes=[BT[5]])
            P.op("pool", lambda e, sq=sq: e.tensor_scalar(out=sq, in0=sq, scalar1=0.044715, scalar2=1.0, op0=ALU.mult, op1=ALU.add), reads=[BT[5]], writes=[BT[5]])
            P.op("pool", lambda e, gb_c=gb_c, sq=sq: e.tensor_tensor(out=sq, in0=sq, in1=gb_c, op=ALU.mult), reads=[BT[5], b_gb], writes=[BT[5]])
        for c in range(4):
            r_ = rt[c][1]; i_ = rt[c][2]; BT = b_rt[c]
            pa = next_ps(4, 7)
            P.op("pe", lambda e, c=c, pa=pa: e.matmul(psum[pa], lhsT=wbd[:, 0, c, :], rhs=xcb[c], start=True, stop=True), reads=[b_wbd, b_xcb[c]], writes=[pbuf[pa]])
            P.op("act", lambda e, c=c, pa=pa, r_=r_: e.activation(out=r_, in_=psum[pa], func=AF.Sigmoid, bias=ba_t[:, c:c + 1]), reads=[pbuf[pa], b_ba], writes=[BT[1]])
            px = next_ps(4, 7)
            P.op("pe", lambda e, c=c, px=px: e.matmul(psum[px], lhsT=wbd[:, 1, c, :], rhs=xcb[c], start=True, stop=True), reads=[b_wbd, b_xcb[c]], writes=[pbuf[px]])
            P.op("act", lambda e, c=c, px=px, i_=i_: e.activation(out=i_, in_=psum[px], func=AF.Sigmoid, bias=bx_t[:, c:c + 1]), reads=[pbuf[px], b_bx], writes=[BT[2]])
        for c in range(4):
            sq = rt[c][5]; ge = rt[c][5]; BT = b_rt[c]; gb_c = gbT[:, c, :]
            P.op("act", lambda e, sq=sq, ge=ge: e.activation(out=ge, in_=sq, func=AF.Sigmoid, scale=1.5957691216057308), reads=[BT[5]], writes=[BT[5]])
            P.op("pool", lambda e, gb_c=gb_c, ge=ge: e.tensor_tensor(out=ge, in0=ge, in1=gb_c, op=ALU.mult), reads=[BT[5], b_gb], writes=[BT[5]])
        for c in range(4):
            r_ = rt[c][1]; a_ = rt[c][3]; a2 = rt[c][4]; BT = b_rt[c]
            P.op("act", lambda e, c=c, r_=r_, a_=a_: e.activation(out=a_, in_=r_, func=AF.Exp, scale=cneg[:, c:c + 1]), reads=[BT[1], b_cn], writes=[BT[3]])
            P.op("act", lambda e, c=c, r_=r_, a2=a2: e.activation(out=a2, in_=r_, func=AF.Exp, scale=c2[:, c:c + 1]), reads=[BT[1], b_cn], writes=[BT[4]])
            P.op("dve", lambda e, a2=a2: e.tensor_scalar(out=a2, in0=a2, scalar1=1.0, scalar2=0.0, op0=ALU.subtract, op1=ALU.min), reads=[BT[4]], writes=[BT[4]])
            xc = rt[c][0]; i_ = rt[c][2]; u_ = rt[c][2]
            P.op("dve", lambda e, i_=i_, xc=xc, u_=u_: e.tensor_tensor(out=u_, in0=i_, in1=xc, op=ALU.mult), reads=[BT[2], BT[0]], writes=[BT[2]])
        for c in range(4):
            a2 = rt[c][4]; BT = b_rt[c]
            P.op("act", lambda e, a2=a2: e.activation(out=a2, in_=a2, func=AF.Sqrt, scale=-1.0), reads=[BT[4]], writes=[BT[4]])
        for c in range(4):
            a_ = rt[c][3]; a2 = rt[c][4]; u_ = rt[c][2]; ge = rt[c][5]; BT = b_rt[c]
            P.op("dve", lambda e, a2=a2, u_=u_: e.tensor_tensor(out=u_, in0=u_, in1=a2, op=ALU.mult), reads=[BT[2], BT[4]], writes=[BT[2]])
            init = 0.0 if tt == 0 else hst[prv][:, c, TT - 1:TT]
            P.op("dve", lambda e, c=c, a_=a_, u_=u_, init=init: e.tensor_tensor_scan(out=hst[cur][:, c, :], data0=a_, data1=u_, initial=init, op0=ALU.mult, op1=ALU.add),
                 reads=[BT[3], BT[2], b_hst[prv]], writes=[b_hst[cur]])
            P.op("dve", lambda e, c=c, ge=ge: e.tensor_tensor(out=recT[:, c, tt * TT:(tt + 1) * TT], in0=hst[cur][:, c, :], in1=ge, op=ALU.mult),
                 reads=[b_hst[cur], BT[5]], writes=[b_recT[tt]])
    P.barrier()
    A.release()

    dump("d_attnT", attnT, b_attnT); dump("d_recT", recT, b_recT)
    P.barrier()
    A.mark()
    w_o = A.bf16(KC * D).rearrange("p (k n) -> p k n", k=KC); b_wo = Buf()
    for kc in range(KC):
        P.dma("sp", w_o[:, kc, :], w_out_b[kc * 128:(kc + 1) * 128, :], reads=[b_woutb[kc]], writes=[b_wo])
    xt = [A.f32(D) for _ in range(3)]; b_xt = [Buf() for _ in range(3)]
    for si in range(NS):
        tt = si // 4
        xi = si % 3
        P.dma("sp", xt[xi], x[si * 128:(si + 1) * 128, :], writes=[b_xt[xi]])
        for d in range(2):
            pi = next_ps(0, 8)
            for kc in range(KC):
                src = attnT[:, kc, si * 128:(si + 1) * 128] if kc < 4 else recT[:, kc - 4, si * 128:(si + 1) * 128]
                bsrc = b_attnT[tt] if kc < 4 else b_recT[tt]
                P.op("pe", lambda e, kc=kc, src=src, d=d, pi=pi: e.matmul(psum[pi], lhsT=src, rhs=w_o[:, kc, d * 512:(d + 1) * 512], start=(kc == 0), stop=(kc == KC - 1)),
                     reads=[bsrc, b_wo], writes=[pbuf[pi]])
            P.op("dve", lambda e, xi=xi, d=d, pi=pi: e.tensor_tensor(out=xt[xi][:, d * 512:(d + 1) * 512], in0=psum[pi], in1=xt[xi][:, d * 512:(d + 1) * 512], op=ALU.add),
                 reads=[pbuf[pi], b_xt[xi]], writes=[b_xt[xi]])
        P.dma("sp", h1d[si * 128:(si + 1) * 128, :], xt[xi], reads=[b_xt[xi]], writes=[h1buf[si]])
    P.barrier()
    A.release()
    A.release()

    if stop_after == "A":
        P.final_wait("sp")
        P.emit()
        return nc

    NJ = (S + TS - 1) // TS
    h3d = dscr("h3d", [S, D], F32); b_h3 = [Buf() for _ in range(NS)]
    xnd = dscr("xnd", [S, D]); b_xnd = [Buf() for _ in range(NS)]
    Yd = dscr("Yd", [NSLOT, D], F32); b_Yd = Buf()

    M1 = A.f32(NS * NE).rearrange("p (s n) -> p s n", s=NS); M2 = A.f32(NS * NE).rearrange("p (s n) -> p s n", s=NS)
    G = A.f32(NS * 2).rearrange("p (s n) -> p s n", s=NS); b_rt_info = Buf()
    sgt = [A.f32(TT) for _ in range(2)]; b_sgt = [Buf() for _ in range(2)]
    gu_rr = [0]

    def swiglu_core(nch, gs, wg_t, wu_t, b_wg, b_wu, dma_gu, dma_wd, nwd, xin, b_xin, hidT, b_hid, wd_of, sink, ntok=TT, hook=None):
        ngrp = nch // gs
        nslots = len(wg_t)
        pend_wd = list(range(nwd))
        for g in range(ngrp):
            sl = gu_rr[0] % nslots; gu_rr[0] += 1
            dma_gu(g, sl)
            if g >= 1 and pend_wd:
                for _ in range(max(1, (nwd + ngrp - 2) // max(1, ngrp - 1))):
                    if pend_wd:
                        dma_wd(pend_wd.pop(0))
            for cc in range(gs):
                c = gs * g + cc
                pg = (c % 2) * 2; pu = pg + 1
                for kc in range(KC):
                    P.op("pe", lambda e, kc=kc, cc=cc, sl=sl, pg=pg: e.matmul(psum[pg][:, 0:ntok], lhsT=wg_t[sl][:, kc, cc * 128:(cc + 1) * 128], rhs=xin[:, kc, :], start=(kc == 0), stop=(kc == KC - 1)),
                         reads=[b_wg[sl], b_xin], writes=[pbuf[pg]])
                for kc in range(KC):
                    P.op("pe", lambda e, kc=kc, cc=cc, sl=sl, pu=pu: e.matmul(psum[pu][:, 0:ntok], lhsT=wu_t[sl][:, kc, cc * 128:(cc + 1) * 128], rhs=xin[:, kc, :], start=(kc == 0), stop=(kc == KC - 1)),
                         reads=[b_wu[sl], b_xin], writes=[pbuf[pu]])
                q = c % 2
                P.op("act", lambda e, q=q, pg=pg: e.activation(out=sgt[q][:, 0:ntok], in_=psum[pg][:, 0:ntok], func=AF.Silu), reads=[pbuf[pg]], writes=[b_sgt[q]])
                P.op("dve", lambda e, q=q, pu=pu, c=c: e.tensor_tensor(out=hidT[:, c, :], in0=sgt[q][:, 0:ntok], in1=psum[pu][:, 0:ntok], op=ALU.mult), reads=[b_sgt[q], pbuf[pu]], writes=[b_hid[c]])
                if hook is not None:
                    hook()
        while pend_wd:
            dma_wd(pend_wd.pop(0))
        k = 0
        for s in range(ntok // 128):
            for d in range(2):
                pd = 4 + (k % 2); k += 1
                for c in range(nch):
                    wap, wbuf = wd_of(c)
                    P.op("pe", lambda e, c=c, s=s, d=d, pd=pd, wap=wap: e.matmul(psum[pd], lhsT=hidT[:, c, s * 128:(s + 1) * 128], rhs=wap[:, d * 512:(d + 1) * 512], start=(c == 0), stop=(c == nch - 1)),
                         reads=[b_hid[c], wbuf], writes=[pbuf[pd]])
                sink(s, d, pd)
                if hook is not None:
                    hook()

    tick(max(0, len(pc_q) - 96))
    A.mark()
    g1_bc, b_g1 = load_bcast(ln_ffn_even, D, "g1")
    g2_bc, b_g2 = load_bcast(ln_mix_odd, D, "g2")
    g3_bc, b_g3 = load_bcast(ln_ffn_odd, D, "g3")
    pw_s = A.bf16(4 * 2 * 256).rearrange("p (g k n) -> p g k n", g=4, k=2); b_pws = Buf()
    A.mark()
    psc_bc, b_psc = load_bcast(pool_scale, D, "psc")
    pw = A.bf16(4 * 2 * 256).rearrange("p (g k n) -> p g k n", g=4, k=2); b_pw = Buf()
    for g in range(4):
        P.dma("sp", pw[:, g, :, :], pool_w_b[g].rearrange("(k p) n -> p k n", p=128), reads=[b_pwb[g]], writes=[b_pw])
    for g in range(4):
        for k2 in range(2):
            P.op("dve", lambda e, g=g, k2=k2: e.tensor_tensor(out=pw_s[:, g, k2, :], in0=pw[:, g, k2, :], in1=psc_bc[:, g * 256:(g + 1) * 256], op=ALU.mult),
                 reads=[b_pw, b_psc], writes=[b_pws])
    P.barrier()
    A.release()
    rw = A.f32(KC * NE).rearrange("p (k n) -> p k n", k=KC); b_rw = Buf()
    P.dma("sp", rw, router_w.rearrange("(k p) n -> p k n", p=128), writes=[b_rw])
    icnt = A.f32(8 * 16).rearrange("p (c t) -> p c t", c=8); b_icnt = Buf()
    P.dma("sp", icnt, invcnt, writes=[b_icnt])

    R2 = [A.f32(4 * D).rearrange("p (s n) -> p s n", s=4) for _ in range(2)]; b_R2 = [[Buf() for _ in range(4)] for _ in range(2)]
    ntmp = norm_tmp(2)
    xnT2 = [A.bf16(KC * TT).rearrange("p (c t) -> p c t", c=KC) for _ in range(2)]; b_xnT2 = [Buf(), Buf()]
    HAL = 16
    xpT = A.bf16(KC * (TT + HAL)).rearrange("p (c t) -> p c t", c=KC); b_xpT = Buf()
    halo = A.bf16(KC * HAL).rearrange("p (c t) -> p c t", c=KC); b_halo = Buf()
    pacc_l = [A.f32(2 * (TT + HAL)).rearrange("p (c t) -> p c t", c=2)] * 2; b_pacc_l = [Buf()] * 2
    pacc2_l = [A.f32(2 * (TT + HAL)).rearrange("p (c t) -> p c t", c=2)] * 2; b_pacc2_l = [Buf()] * 2
    dT = A.bf16(KC * TT).rearrange("p (c t) -> p c t", c=KC); b_dT = Buf()
    xn32 = A.f32(D); b_xn32 = Buf()
    xnb = [A.bf16(D)] * 2; b_xnb = [Buf()] * 2
    xT32 = A.f32(KC * 128).rearrange("p (c t) -> p c t", c=KC); b_xT32 = Buf()
    lg = A.f32(8); b_lg = Buf()
    top8 = A.f32(8); b_top8 = Buf()
    gt = A.f32(2); b_m = Buf()
    NCF = FFN // 128
    NGU = 3
    wg_t = [A.bf16(KC * 256).rearrange("p (k n) -> p k n", k=KC) for _ in range(NGU)]; b_wg = [Buf() for _ in range(NGU)]
    wu_t = [A.bf16(KC * 256).rearrange("p (k n) -> p k n", k=KC) for _ in range(NGU)]; b_wu = [Buf() for _ in range(NGU)]
    hidF = A.bf16(NCF * TT).rearrange("p (c t) -> p c t", c=NCF); b_hidF = [Buf() for _ in range(NCF)]
    wdF = A.bf16(NCF * D).rearrange("p (c n) -> p c n", c=NCF); b_wdF = [Buf() for _ in range(NCF // 2)]
    P.op("pool", lambda e: e.memset(halo.rearrange("p c t -> p (c t)"), 0.0), writes=[b_halo])

    def ffn_gu(g, sl):
        P.dma("sp", wg_t[sl].rearrange("p k n -> p (k n)"), ffn_gq[g], reads=[b_fg[g]], writes=[b_wg[sl]])
        P.dma("sp", wu_t[sl].rearrange("p k n -> p (k n)"), ffn_uq[g], reads=[b_fu[g]], writes=[b_wu[sl]])

    def ffn_wd(g):
        P.dma("sp", wdF[:, 2 * g:2 * g + 2, :].rearrange("p c n -> p (c n)"), ffn_dq[g], reads=[b_fd[g]], writes=[b_wdF[g]])

    POOLW = (2, 4, 8, 16)
    W = TT + HAL

    def tail(tt):
        R = R2[tt % 2]; b_R = b_R2[tt % 2]
        P.op("pool", lambda e: e.tensor_copy(out=xpT[:, :, 0:HAL], in_=halo), reads=[b_halo], writes=[b_xpT])
        pend = []
        for s in range(4):
            pend.append(norm_T(R[:, s, :], b_R[s], g2_bc, b_g2, xpT, b_xpT, HAL + s * 128, ntmp, 7, split=True))
            yield
            if len(pend) > 1:
                pend.pop(0)()
            yield
        while pend:
            yield
            pend.pop(0)()
        yield
        P.op("pool", lambda e: e.tensor_copy(out=halo, in_=xpT[:, :, TT:TT + HAL]), reads=[b_xpT], writes=[b_halo])
        for g in range(4):
            cs = slice(2 * g, 2 * g + 2)
            nlev = g + 1
            pacc, b_pacc = pacc_l[g % 2], b_pacc_l[g % 2]
            pacc2, b_pacc2 = pacc2_l[g % 2], b_pacc2_l[g % 2]
            eng = "dve" if g % 2 == 0 else "pool"
            srcap = xpT[:, cs, :]
            bsrc = b_xpT
            for lv_ in range(nlev):
                sh = 1 << lv_
                dst, bdst = (pacc, b_pacc) if lv_ % 2 == 0 else (pacc2, b_pacc2)
                P.op(eng, lambda e, dst=dst, srcap=srcap, sh=sh: e.tensor_tensor(out=dst[:, :, sh:W], in0=srcap[:, :, sh:W], in1=srcap[:, :, 0:W - sh], op=ALU.add),
                     reads=[bsrc], writes=[bdst])
                srcap, bsrc = dst, bdst
                yield
            w = POOLW[g]
            P.op("dve", lambda e, srcap=srcap, cs=cs, w=w: e.scalar_tensor_tensor(out=dT[:, cs, :], in0=srcap[:, :, HAL:W], scalar=1.0 / w, in1=xpT[:, cs, HAL:W], op0=ALU.mult, op1=ALU.subtract),
                 reads=[bsrc, b_xpT], writes=[b_dT])
            if tt == 0:
                tmpap, btmp = (pacc2, b_pacc2) if srcap is pacc else (pacc, b_pacc)
                P.op("dve", lambda e, srcap=srcap, cs=cs, tmpap=tmpap: e.tensor_tensor(out=tmpap[:, :, 0:16], in0=srcap[:, :, HAL:HAL + 16], in1=icnt[:, cs, :], op=ALU.mult),
                     reads=[bsrc, b_icnt], writes=[btmp])
                P.op("dve", lambda e, tmpap=tmpap, cs=cs: e.tensor_tensor(out=dT[:, cs, 0:16], in0=tmpap[:, :, 0:16], in1=xpT[:, cs, HAL:HAL + 16], op=ALU.subtract),
                     reads=[btmp, b_xpT, b_dT], writes=[b_dT])
            yield
        for s in range(4):
            for gp in range(2):
                for g in (2 * gp, 2 * gp + 1):
                    for k2 in range(2):
                        P.op("pe", lambda e, s=s, g=g, k2=k2: e.matmul(psum[6][:, (g % 2) * 256:(g % 2) * 256 + 256], lhsT=dT[:, 2 * g + k2, s * 128:(s + 1) * 128], rhs=pw_s[:, g, k2, :], start=(k2 == 0), stop=(k2 == 1)),
                             reads=[b_dT, b_pws], writes=[pbuf[6]])
                P.op("dve", lambda e, s=s, gp=gp: e.tensor_tensor(out=R[:, s, gp * 512:(gp + 1) * 512], in0=psum[6], in1=R[:, s, gp * 512:(gp + 1) * 512], op=ALU.add),
                     reads=[pbuf[6], b_R[s]], writes=[b_R[s]])
                yield
        junk, ss, rstd, xn_unused, b_tmp = ntmp[0]
        for s in range(4):
            si = tt * 4 + s
            xi = si % 2
            P.dma("pool", h3d[si * 128:(si + 1) * 128, :], R[:, s, :], reads=[b_R[s]], writes=[b_h3[si]])
            P.op("act", lambda e, s=s: e.activation(out=junk, in_=R[:, s, :], func=AF.Square, accum_out=ss), reads=[b_R[s]], writes=[b_tmp[0]])
            P.op("dve", lambda e: e.tensor_scalar(out=rstd, in0=ss, scalar1=1.0 / D, scalar2=NORM_EPS, op0=ALU.mult, op1=ALU.add), reads=[b_tmp[0]], writes=[b_tmp[1]])
            P.op("pool", lambda e: e.tensor_tensor(out=rstd, in0=rstd, in1=cst[:, 0:1], op=ALU.pow), reads=[b_tmp[1], b_cst], writes=[b_tmp[1]])
            tick()
            P.op("dve", lambda e, s=s: e.scalar_tensor_tensor(out=xn32, in0=R[:, s, :], scalar=rstd, in1=g3_bc, op0=ALU.mult, op1=ALU.mult),
                 reads=[b_R[s], b_tmp[1], b_g3], writes=[b_xn32])
            P.op("act", lambda e, xi=xi: e.activation(out=xnb[xi], in_=xn32, func=AF.Copy), reads=[b_xn32], writes=[b_xnb[xi]])
            P.dma("pool", xnd[si * 128:(si + 1) * 128, :], xnb[xi], reads=[b_xnb[xi]], writes=[b_xnd[si]])
            yield
            yield
            for hh in range(2):
                pp = psum[6]
                for c4 in range(4):
                    c = hh * 4 + c4
                    P.op("pe", lambda e, c=c, c4=c4, pp=pp: e.transpose(out=pp[:, c4 * 128:(c4 + 1) * 128], in_=xn32[:, c * 128:(c + 1) * 128], identity=identf),
                         reads=[b_xn32, b_identf], writes=[pbuf[6]])
                P.op("dve", lambda e, hh=hh, pp=pp: e.tensor_copy(out=xT32[:, hh * 4:(hh + 1) * 4, :], in_=pp.rearrange("p (c t) -> p c t", c=4)),
                     reads=[pbuf[6]], writes=[b_xT32])
            yield
            for kc in range(KC):
                P.op("pe", lambda e, kc=kc: e.matmul(psum[6][:, 0:NE], lhsT=xT32[:, kc, :], rhs=rw[:, kc, :], start=(kc == 0), stop=(kc == KC - 1)),
                     reads=[b_xT32, b_rw], writes=[pbuf[6]])
            P.op("dve", lambda e: e.tensor_copy(out=lg, in_=psum[6][:, 0:NE]), reads=[pbuf[6]], writes=[b_lg])
            P.op("dve", lambda e: e.max(out=top8, in_=lg), reads=[b_lg], writes=[b_top8])
            P.op("dve", lambda e, si=si: e.tensor_scalar(out=M1[:, si, :], in0=lg, scalar1=top8[:, 0:1], scalar2=None, op0=ALU.is_equal), reads=[b_lg, b_top8], writes=[b_rt_info])
            P.op("dve", lambda e, si=si: e.tensor_scalar(out=M2[:, si, :], in0=lg, scalar1=top8[:, 1:2], scalar2=None, op0=ALU.is_equal), reads=[b_lg, b_top8], writes=[b_rt_info])
            P.op("dve", lambda e: e.tensor_tensor(out=gt[:, 0:1], in0=top8[:, 1:2], in1=top8[:, 0:1], op=ALU.subtract), reads=[b_top8], writes=[b_m])
            P.op("act", lambda e: e.activation(out=gt[:, 0:1], in_=gt[:, 0:1], func=AF.Exp), reads=[b_m], writes=[b_m])
            P.op("dve", lambda e: e.tensor_scalar(out=gt[:, 0:1], in0=gt[:, 0:1], scalar1=1.0, scalar2=None, op0=ALU.add), reads=[b_m], writes=[b_m])
            P.op("dve", lambda e, si=si: e.reciprocal(out=G[:, si, 0:1], in_=gt[:, 0:1]), reads=[b_m], writes=[b_rt_info])
            P.op("dve", lambda e, si=si: e.tensor_scalar(out=G[:, si, 1:2], in0=G[:, si, 0:1], scalar1=-1.0, scalar2=1.0, op0=ALU.mult, op1=ALU.add), reads=[b_rt_info], writes=[b_rt_info])
            yield

    def prep(tt):
        R = R2[tt % 2]; b_R = b_R2[tt % 2]
        pend = None
        for s in range(4):
            si = tt * 4 + s
            P.dma("pool", R[:, s, :], h1d[si * 128:(si + 1) * 128, :], reads=[h1buf[si]], writes=[b_R[s]])
        yield
        pend = []
        for s in range(4):
            pend.append(norm_T(R[:, s, :], b_R[s], g1_bc, b_g1, xnT2[tt % 2], b_xnT2[tt % 2], s * 128, ntmp, 7, split=True))
            yield
            if len(pend) > 1:
                pend.pop(0)()
            yield
        while pend:
            yield
            pend.pop(0)()
        yield

    def chain(gens):
        for g in gens:
            yield from g

    prev_tail = [None]

    def advance(n):
        g = prev_tail[0]
        if g is None:
            return
        for _ in range(n):
            try:
                next(g)
            except StopIteration:
                prev_tail[0] = None
                return

    for _ in prep(0):
        pass
    for tt in range(NT):
        R = R2[tt % 2]; b_R = b_R2[tt % 2]
        gens = []
        if tt > 0:
            gens.append(tail(tt - 1))
        if tt + 1 < NT:
            gens.append(prep(tt + 1))
        prev_tail[0] = chain(gens)

        def add_plain(s, d, pd, R=R, b_R=b_R):
            P.op("dve", lambda e: e.tensor_tensor(out=R[:, s, d * 512:(d + 1) * 512], in0=psum[pd], in1=R[:, s, d * 512:(d + 1) * 512], op=ALU.add),
                 reads=[pbuf[pd], b_R[s]], writes=[b_R[s]])

        swiglu_core(NCF, 2, wg_t, wu_t, b_wg, b_wu, ffn_gu, ffn_wd, NCF // 2, xnT2[tt % 2], b_xnT2[tt % 2], hidF, b_hidF, lambda c: (wdF[:, c, :], b_wdF[c // 2]), add_plain,
                    hook=lambda: advance(3))
        advance(10 ** 6)
    for _ in tail(NT - 1):
        pass
    tick(len(pc_q))
    P.barrier()
    A.release()

    A.mark()
    NSE = NS * NE
    triu = A.bf16(128); b_triu = Buf()
    P.op("pool", lambda e: e.memset(triu, 1.0), writes=[b_triu])
    P.op("pool", lambda e: e.affine_select(out=triu, in_=triu, pattern=[[1, 128]], compare_op=ALU.is_gt, fill=0.0, base=0, channel_multiplier=-1),
         reads=[b_triu], writes=[b_triu])
    thi = A.f32(NSLOT_T + 1).bitcast(I32); thj = A.f32(NSLOT_T + 1); zer = A.f32(max(NS, 8)); b_th = Buf()
    th8 = thj[:, 0:NJ]
    P.op("pool", lambda e: e.iota(thi[:, 0:NSLOT_T], pattern=[[TS, NSLOT_T]], base=0, channel_multiplier=0), writes=[b_th])
    P.op("dve", lambda e: e.tensor_copy(out=thj[:, 0:NSLOT_T], in_=thi[:, 0:NSLOT_T]), reads=[b_th], writes=[b_th])
    P.op("pool", lambda e: e.memset(zer, 0.0), writes=[b_th])
    CNT = A.f32(NSE); CNTb = A.bf16(NSE); b_cnt = Buf()
    M1f = M1.rearrange("p s n -> p (s n)"); M2f = M2.rearrange("p s n -> p (s n)")
    P.op("dve", lambda e: e.tensor_tensor(out=CNT, in0=M1f, in1=M2f, op=ALU.add), reads=[b_rt_info], writes=[b_cnt])
    P.op("dve", lambda e: e.tensor_copy(out=CNTb, in_=CNT), reads=[b_cnt], writes=[b_cnt])
    P.op("pe", lambda e: e.matmul(psum[0][:, 0:NSE], lhsT=triu, rhs=CNTb, start=True, stop=True), reads=[b_triu, b_cnt], writes=[pbuf[0]])
    P.op("pe", lambda e: e.matmul(psum[1][:, 0:NSE], lhsT=ones_bf, rhs=CNTb, start=True, stop=True), reads=[b_ones, b_cnt], writes=[pbuf[1]])
    TOTe = A.f32(NSE).rearrange("p (n s) -> p n s", n=NE); CUM = A.f32(NSE).rearrange("p (n s) -> p n s", n=NE)
    BO = A.f32(NSE).rearrange("p (n s) -> p n s", n=NE); b_rt2 = Buf()
    P.op("dve", lambda e: e.tensor_copy(out=TOTe, in_=psum[1][:, 0:NSE].rearrange("p (s n) -> p n s", n=NE)), reads=[pbuf[1]], writes=[b_rt2])
    for ex in range(NE):
        P.op("dve", lambda e, ex=ex: e.tensor_tensor_scan(out=CUM[:, ex, :], data0=TOTe[:, ex, :], data1=zer[:, 0:NS], initial=0.0, op0=ALU.add, op1=ALU.add),
             reads=[b_rt2, b_th], writes=[b_rt2])
    P.op("dve", lambda e: e.tensor_tensor(out=BO, in0=CUM, in1=TOTe, op=ALU.subtract), reads=[b_rt2], writes=[b_rt2])
    cmp8 = A.f32(NE * NJ).rearrange("p (n j) -> p n j", n=NE)
    PSz = A.f32(8); CUMPS = A.f32(8); OFF = A.f32(8)
    for ex in range(NE):
        P.op("dve", lambda e, ex=ex: e.tensor_scalar(out=cmp8[:, ex, :], in0=th8, scalar1=CUM[:, ex, NS - 1:NS], scalar2=None, op0=ALU.is_lt),
             reads=[b_rt2, b_th], writes=[b_rt2])
    P.op("dve", lambda e: e.tensor_reduce(out=PSz, in_=cmp8, axis=mybir.AxisListType.X, op=ALU.add), reads=[b_rt2], writes=[b_rt2])
    P.op("dve", lambda e: e.tensor_scalar(out=PSz, in0=PSz, scalar1=float(TS), scalar2=None, op0=ALU.mult), reads=[b_rt2], writes=[b_rt2])
    P.op("dve", lambda e: e.tensor_tensor_scan(out=CUMPS, data0=PSz, data1=zer[:, 0:8], initial=0.0, op0=ALU.add, op1=ALU.add), reads=[b_rt2, b_th], writes=[b_rt2])
    P.op("dve", lambda e: e.tensor_tensor(out=OFF, in0=CUMPS, in1=PSz, op=ALU.subtract), reads=[b_rt2], writes=[b_rt2])
    for ex in range(NE):
        P.op("dve", lambda e, ex=ex: e.tensor_scalar(out=BO[:, ex, :], in0=BO[:, ex, :], scalar1=OFF[:, ex:ex + 1], scalar2=None, op0=ALU.add),
             reads=[b_rt2], writes=[b_rt2])
    SLOT = A.f32(NSE).rearrange("p (s n) -> p s n", s=NS); STMP = A.f32(NSE).rearrange("p (s n) -> p s n", s=NS)
    P.op("dve", lambda e: e.tensor_tensor(out=SLOT, in0=psum[0][:, 0:NSE].rearrange("p (s n) -> p s n", n=NE), in1=BO.rearrange("p n s -> p s n"), op=ALU.add),
         reads=[pbuf[0], b_rt2], writes=[b_rt2])
    posf = A.f32(2 * NS).rearrange("p (k s) -> p k s", k=2); posi_w = A.f32(2 * NS); posi = posi_w.bitcast(I32).rearrange("p (k s) -> p k s", k=2); b_pos = Buf()
    for k_, Mk in enumerate((M1, M2)):
        P.op("dve", lambda e, Mk=Mk: e.tensor_tensor(out=STMP, in0=Mk, in1=SLOT, op=ALU.mult), reads=[b_rt_info, b_rt2], writes=[b_rt2])
        P.op("dve", lambda e, k_=k_: e.tensor_reduce(out=posf[:, k_, :], in_=STMP, axis=mybir.AxisListType.X, op=ALU.add), reads=[b_rt2], writes=[b_pos])
    P.op("dve", lambda e: e.tensor_copy(out=posi.rearrange("p k s -> p (k s)"), in_=posf.rearrange("p k s -> p (k s)")), reads=[b_pos], writes=[b_pos])
    cmpj = A.f32(NE * NSLOT_T).rearrange("p (n j) -> p n j", n=NE)
    EJf = A.f32(NSLOT_T + 1); EJi = A.f32(NSLOT_T + 1).bitcast(I32); b_ej = Buf()
    for ex in range(NE):
        P.op("dve", lambda e, ex=ex: e.tensor_scalar(out=cmpj[:, ex, :], in0=thj[:, 0:NSLOT_T], scalar1=CUMPS[:, ex:ex + 1], scalar2=None, op0=ALU.is_ge),
             reads=[b_rt2, b_th], writes=[b_rt2])
    P.op("dve", lambda e: e.tensor_reduce(out=EJf[:, 0:NSLOT_T], in_=cmpj.rearrange("p n j -> p j n"), axis=mybir.AxisListType.X, op=ALU.add), reads=[b_rt2], writes=[b_ej])
    P.op("dve", lambda e: e.tensor_scalar(out=EJf[:, 0:NSLOT_T], in0=EJf[:, 0:NSLOT_T], scalar1=float(NE - 1), scalar2=None, op0=ALU.min), reads=[b_ej], writes=[b_ej])
    P.op("dve", lambda e: e.tensor_copy(out=EJi[:, 0:NSLOT_T], in_=EJf[:, 0:NSLOT_T]), reads=[b_ej], writes=[b_ej])
    piota_i = A.f32(2).bitcast(I32); piota = A.f32(2); idxwf = A.f32(NSLOT_T + 1); idxw = A.f32(NSLOT_T + 1).bitcast(I32); b_idxw = Buf()
    P.op("pool", lambda e: e.iota(piota_i[:, 0:1], pattern=[[0, 1]], base=0, channel_multiplier=1), writes=[b_idxw])
    P.op("dve", lambda e: e.tensor_copy(out=piota[:, 0:1], in_=piota_i[:, 0:1]), reads=[b_idxw], writes=[b_idxw])
    P.op("dve", lambda e: e.tensor_scalar(out=idxwf[:, 0:NSLOT_T], in0=EJf[:, 0:NSLOT_T], scalar1=128.0, scalar2=piota[:, 0:1], op0=ALU.mult, op1=ALU.add),
         reads=[b_ej, b_idxw], writes=[b_idxw])
    P.op("dve", lambda e: e.tensor_copy(out=idxw[:, 0:NSLOT_T], in_=idxwf[:, 0:NSLOT_T]), reads=[b_idxw], writes=[b_idxw])
    if dbg:
        dump("d_posf", posf, [b_pos], F32); dump("d_ejf", EJf, [b_ej], F32); dump("d_M1", M1, [b_rt_info], F32); dump("d_M2", M2, [b_rt_info], F32)
        dump("d_G", G, [b_rt_info], F32)

    A.mark()
    xs_t = [A.bf16(D) for _ in range(8)]; b_xs = [Buf() for _ in range(8)]
    for si in range(NS):
        i3 = si % 8
        P.dma("sp", xs_t[i3], xnd[si * 128:(si + 1) * 128, :], reads=[b_xnd[si]], writes=[b_xs[i3]])
        for k_ in range(2):
            P.dma_custom("pool", lambda e, si=si, k_=k_, i3=i3: e.indirect_dma_start(
                out=Xg, out_offset=bass.IndirectOffsetOnAxis(ap=posi[:, k_, si:si + 1], axis=0), in_=xs_t[i3], in_offset=None),
                reads=[b_xs[i3], b_pos], writes=[b_Xg])
    P.barrier()
    A.release()

    A.mark()
    NCH = EXP // 128
    XgT = [A.bf16(KC * TS).rearrange("p (c t) -> p c t", c=KC) for _ in range(2)]; b_XgT = [Buf() for _ in range(2)]
    xgt = [A.bf16(D) for _ in range(4)]; b_xgt = [Buf() for _ in range(4)]
    hidT = A.bf16(NCH * TS).rearrange("p (c t) -> p c t", c=NCH); b_hid = [Buf() for _ in range(NCH)]
    QW = EXP // 4
    wgq_t = [A.bf16(KC * QW).rearrange("p (k n) -> p k n", k=KC) for _ in range(2)]; b_wgq = [Buf() for _ in range(2)]
    wuq_t = [A.bf16(KC * QW).rearrange("p (k n) -> p k n", k=KC) for _ in range(2)]; b_wuq = [Buf() for _ in range(2)]
    wdq_t = [A.bf16(7 * D).rearrange("p (c n) -> p c n", c=7) for _ in range(4)]; b_wdq = [Buf() for _ in range(4)]
    Yt = [A.f32(D) for _ in range(2)]; b_Yt = [Buf() for _ in range(2)]

    NSUB = TS // 128

    def load_xg_dma(j):
        for s in range(NSUB):
            q4 = (j * NSUB + s) % 4
            P.dma("sp", xgt[q4], Xg[j * TS + s * 128:j * TS + (s + 1) * 128, :], reads=[b_Xg], writes=[b_xgt[q4]])

    def load_xg_tr(j):
        xb_ = j % 2
        for s in range(NSUB):
            q4 = (j * NSUB + s) % 4
            pb = psum[7].bitcast(BF16)
            for c in range(KC):
                P.op("pe", lambda e, c=c, q4=q4, pb=pb: e.transpose(out=pb[:, c * 128:(c + 1) * 128], in_=xgt[q4][:, c * 128:(c + 1) * 128], identity=ident),
                     reads=[b_xgt[q4], b_ident], writes=[pbuf[7]])
            P.op("act", lambda e, s=s, xb_=xb_, pb=pb: e.activation(out=XgT[xb_][:, :, s * 128:(s + 1) * 128], in_=pb.rearrange("p (c t) -> p c t", c=KC), func=AF.Copy),
                 reads=[pbuf[7]], writes=[b_XgT[xb_]])

    load_xg_dma(0)
    load_xg_tr(0)
    for j in range(NSLOT_T):
        xb_ = j % 2

        def moe_gu(g, sl, j=j):
            P.dma_custom("pool", lambda e, g=g, sl=sl, j=j: e.indirect_dma_start(
                out=wgq_t[sl].rearrange("p k n -> p (k n)"), out_offset=None, in_=moe_gq[g],
                in_offset=bass.IndirectOffsetOnAxis(ap=idxw[:, j:j + 1], axis=0)), reads=[b_idxw] + b_moe, writes=[b_wgq[sl]])
            P.dma_custom("pool", lambda e, g=g, sl=sl, j=j: e.indirect_dma_start(
                out=wuq_t[sl].rearrange("p k n -> p (k n)"), out_offset=None, in_=moe_uq[g],
                in_offset=bass.IndirectOffsetOnAxis(ap=idxw[:, j:j + 1], axis=0)), reads=[b_idxw] + b_moe, writes=[b_wuq[sl]])

        def moe_wd(g, j=j):
            P.dma_custom("pool", lambda e, g=g, j=j: e.indirect_dma_start(
                out=wdq_t[g].rearrange("p c n -> p (c n)"), out_offset=None, in_=moe_dq[g],
                in_offset=bass.IndirectOffsetOnAxis(ap=idxw[:, j:j + 1], axis=0)), reads=[b_idxw] + b_moe, writes=[b_wdq[g]])

        def sink(s, d, pd, j=j):
            yb = s % 2
            if d == 0:
                P.op("act", lambda e: e.activation(out=Yt[yb][:, d * 512:(d + 1) * 512], in_=psum[pd], func=AF.Copy), reads=[pbuf[pd]], writes=[b_Yt[yb]])
            else:
                P.op("dve", lambda e: e.tensor_copy(out=Yt[yb][:, d * 512:(d + 1) * 512], in_=psum[pd]), reads=[pbuf[pd]], writes=[b_Yt[yb]])
                P.dma("sp", Yd[j * TS + s * 128:j * TS + (s + 1) * 128, :], Yt[yb], reads=[b_Yt[yb]], writes=[b_Yd])

        hk = [0]

        def hook(j=j, hk=hk):
            hk[0] += 1
            if j + 1 < NSLOT_T:
                if hk[0] == 2:
                    load_xg_dma(j + 1)
                elif hk[0] == 12:
                    load_xg_tr(j + 1)

        swiglu_core(NCH, 7, wgq_t, wuq_t, b_wgq, b_wuq, moe_gu, moe_wd, 4, XgT[xb_], b_XgT[xb_], hidT, b_hid,
                    lambda c: (wdq_t[c // 7][:, c % 7, :], b_wdq[c // 7]), sink, ntok=TS, hook=hook)
    P.barrier()
    A.release()

    A.mark()
    NB5 = 6
    Y1 = [A.f32(D) for _ in range(NB5)]; Y2 = [A.f32(D) for _ in range(NB5)]; Rt = [A.f32(D) for _ in range(NB5)]
    b_Y1 = [Buf() for _ in range(NB5)]; b_Y2 = [Buf() for _ in range(NB5)]; b_Rt = [Buf() for _ in range(NB5)]
    ot = [A.f32(D) for _ in range(NB5)]; b_ot = [Buf() for _ in range(NB5)]
    ntmp = norm_tmp()
    g4_bc, b_g4 = load_bcast(final_g, D, "g4")
    junk, ss, rstd, xn_unused, b_tmp = ntmp
    for si in range(NS):
        i2 = si % NB5
        P.dma_custom("pool", lambda e, si=si, i2=i2: e.indirect_dma_start(
            out=Y1[i2], out_offset=None, in_=Yd, in_offset=bass.IndirectOffsetOnAxis(ap=posi[:, 0, si:si + 1], axis=0)),
            reads=[b_Yd, b_pos], writes=[b_Y1[i2]])
        P.dma_custom("pool", lambda e, si=si, i2=i2: e.indirect_dma_start(
            out=Y2[i2], out_offset=None, in_=Yd, in_offset=bass.IndirectOffsetOnAxis(ap=posi[:, 1, si:si + 1], axis=0)),
            reads=[b_Yd, b_pos], writes=[b_Y2[i2]])
        P.dma("sp", Rt[i2], h3d[si * 128:(si + 1) * 128, :], reads=[b_h3[si]], writes=[b_Rt[i2]])
        P.op("dve", lambda e, si=si, i2=i2: e.scalar_tensor_tensor(out=Rt[i2], in0=Y1[i2], scalar=G[:, si, 0:1], in1=Rt[i2], op0=ALU.mult, op1=ALU.add),
             reads=[b_Y1[i2], b_Rt[i2], b_rt_info], writes=[b_Rt[i2]])
        P.op("dve", lambda e, si=si, i2=i2: e.scalar_tensor_tensor(out=Rt[i2], in0=Y2[i2], scalar=G[:, si, 1:2], in1=Rt[i2], op0=ALU.mult, op1=ALU.add),
             reads=[b_Y2[i2], b_Rt[i2], b_rt_info], writes=[b_Rt[i2]])
        P.op("act", lambda e, i2=i2: e.activation(out=junk, in_=Rt[i2], func=AF.Square, accum_out=ss), reads=[b_Rt[i2]], writes=[b_tmp[0]])
        P.op("act", lambda e: e.activation(out=rstd, in_=ss, func=AF.Ln, scale=1.0 / D, bias=cst[:, 1:2]), reads=[b_tmp[0], b_cst], writes=[b_tmp[1]])
        P.op("act", lambda e: e.activation(out=rstd, in_=rstd, func=AF.Exp, scale=-0.5), reads=[b_tmp[1]], writes=[b_tmp[1]])
        P.op("dve", lambda e, i2=i2: e.scalar_tensor_tensor(out=ot[i2], in0=Rt[i2], scalar=rstd, in1=g4_bc, op0=ALU.mult, op1=ALU.mult),
             reads=[b_Rt[i2], b_tmp[1], b_g4], writes=[b_ot[i2]])
        P.dma("sp", y[si * 128:(si + 1) * 128, :], ot[i2], reads=[b_ot[i2]], writes=[ybuf])
    P.final_wait("sp")
    P.emit()
    return nc


def _consts(S):
    half = 8
    inv = 500000.0 ** (-np.arange(0, 16, 2, dtype=np.float32) / 16.0)
    ang = np.arange(S, dtype=np.float32)[:, None] * inv[None, :]
    cos = np.cos(ang).astype(np.float32).T
    sin = np.sin(ang).astype(np.float32).T
    C = np.ones((128, S), np.float32); Sg = np.zeros((128, S), np.float32)
    for m in range(2):
        b = m * 64
        C[b:b + 8] = cos; C[b + 8:b + 16] = cos
        Sg[b:b + 8] = -sin; Sg[b + 8:b + 16] = sin
    ropet = np.stack([C * 0.125, Sg * 0.125, C, Sg]).astype(np.float32)
    invcnt = np.zeros((128, 8, 16), np.float32)
    for g, w in enumerate((2, 4, 8, 16)):
        v = 1.0 / np.minimum(np.arange(16) + 1, w).astype(np.float32)
        invcnt[:, 2 * g] = v; invcnt[:, 2 * g + 1] = v
    return ropet, invcnt


_NC_CACHE = {}


def make_in_maps(inputs, S, nb):
    ropet, invcnt = _consts(S)
    sq = lambda a: np.ascontiguousarray(np.asarray(a)[0])
    shared = {
        "ln_mix_even": sq(inputs["ln_mix_even"]), "ln_ffn_even": sq(inputs["ln_ffn_even"]),
        "ln_mix_odd": sq(inputs["ln_mix_odd"]), "ln_ffn_odd": sq(inputs["ln_ffn_odd"]),
        "final_g": np.ascontiguousarray(np.asarray(inputs["final_g"])), "pool_scale": sq(inputs["pool_scale"]),
        "w_in_even": sq(inputs["w_in_even"]), "w_out_even": sq(inputs["w_out_even"]),
        "lam_q1": sq(inputs["lam_q1"]), "lam_k1": sq(inputs["lam_k1"]), "lam_q2": sq(inputs["lam_q2"]), "lam_k2": sq(inputs["lam_k2"]),
        "subln_g": sq(inputs["subln_g"]), "conv_w": sq(inputs["conv_w"]), "conv_b": sq(inputs["conv_b"]),
        "rg_wa": sq(inputs["rg_wa"]), "rg_ba": sq(inputs["rg_ba"]).reshape(512),
        "rg_wx": sq(inputs["rg_wx"]), "rg_bx": sq(inputs["rg_bx"]).reshape(512), "rg_lam": sq(inputs["rg_lam"]),
        "ffn_wg": sq(inputs["ffn_wg"]), "ffn_wu": sq(inputs["ffn_wu"]), "ffn_wd": sq(inputs["ffn_wd"]),
        "pool_w": sq(inputs["pool_w"]), "router_w": sq(inputs["router_w"]),
        "moe_wg": sq(inputs["moe_wg"]), "moe_wu": sq(inputs["moe_wu"]), "moe_wd": sq(inputs["moe_wd"]),
        "ropet": ropet, "invcnt": invcnt,
    }
    xs = np.asarray(inputs["x"])
    maps = []
    for b in range(nb):
        m = dict(shared)
        m["x"] = np.ascontiguousarray(xs[b])
        maps.append(m)
    return maps


def kernel(**inputs):
    x = np.asarray(inputs["x"])
    B, S, _ = x.shape
    if S not in _NC_CACHE:
        _NC_CACHE[S] = build(S)
    nc = _NC_CACHE[S]
    in_maps = make_in_maps(inputs, S, B)
    res = run_bass_kernel_spmd(nc, in_maps, core_ids=list(range(B)))
    return np.stack([np.asarray(r["y"]) for r in res.results], axis=0).astype(np.float32)
```

```python
import contextlib
import math
import numpy as np
import concourse.bass as bass
import concourse.mybir as mybir
from concourse.bass_utils import run_bass_kernel_spmd

F32 = mybir.dt.float32
BF16 = mybir.dt.bfloat16
I32 = mybir.dt.int32
AF = mybir.ActivationFunctionType
ALU = mybir.AluOpType

ENGS = ("pe", "act", "dve", "pool", "sp")

D = 1024
KC = 8
IN_W = 2560
FFN = 2816
NE = 8
EXP = 3584
NORM_EPS = 1e-6
SUBLN_EPS = 1e-5
LAM_INIT0 = 0.8 - 0.6 * math.exp(-0.3 * 0)
TT = 512


class Buf:
    __slots__ = ("name", "last_write", "readers")

    def __init__(self, name=""):
        self.name = name
        self.last_write = None
        self.readers = {}


class Op:
    __slots__ = ("eng", "fn", "deps", "is_dma", "semkey", "ticket", "needed", "inc")

    def __init__(self, eng, fn, is_dma=False, semkey=None):
        self.eng = eng
        self.fn = fn
        self.deps = []
        self.is_dma = is_dma
        self.semkey = semkey if semkey is not None else eng
        self.ticket = None
        self.needed = False
        self.inc = 16 if is_dma else 1


class _Rec:
    def __init__(self):
        self.call = None

    def __getattr__(self, name):
        def f(*a, **k):
            self.call = (name, a, k)
            return None
        return f


def _bind(fn):
    rec = _Rec()
    fn(rec)
    name, a, k = rec.call
    return lambda e: getattr(e, name)(*a, **k)


class Prog:
    def __init__(self, nc, n_dma_sems=32):
        self.nc = nc
        self.ops = {e: [] for e in ENGS}
        self.all_ops = []
        self.last_op_per_sem = {}
        self.dma_cnt = {}

    def _deps(self, op, reads, writes):
        deps = []
        for b in reads:
            if b.last_write is not None:
                deps.append(b.last_write)
        for b in writes:
            if b.last_write is not None:
                deps.append(b.last_write)
            deps.extend(b.readers.values())
        out = []
        for d in deps:
            if d is op:
                continue
            if d.eng == op.eng and op.eng == "pe" and not d.is_dma and not op.is_dma:
                continue
            out.append(d)
        op.deps = out
        for d in out:
            d.needed = True
        for b in reads:
            b.readers[op.semkey] = op
        for b in writes:
            b.last_write = op
            b.readers = {}

    def op(self, eng, fn, reads=(), writes=()):
        o = Op(eng, _bind(fn))
        self._deps(o, reads, writes)
        self.ops[eng].append(o)
        self.all_ops.append(o)
        self.last_op_per_sem[o.semkey] = o
        return o

    def dma(self, eng, out_ap, in_ap, reads=(), writes=(), **kw):
        k = self._dma_key(eng)
        fn = lambda e: e.dma_start(out=out_ap, in_=in_ap, **kw)
        o = Op(eng, fn, is_dma=True, semkey=k)
        self._deps(o, reads, writes)
        prev = self.last_op_per_sem.get(k)
        if prev is not None:
            o.deps.append(prev)
            prev.needed = True
        self.ops[eng].append(o)
        self.all_ops.append(o)
        self.last_op_per_sem[k] = o
        return o

    def dma_custom(self, eng, fn, reads=(), writes=()):
        k = self._dma_key(eng)
        o = Op(eng, fn, is_dma=True, semkey=k)
        self._deps(o, reads, writes)
        prev = self.last_op_per_sem.get(k)
        if prev is not None:
            o.deps.append(prev)
            prev.needed = True
        self.ops[eng].append(o)
        self.all_ops.append(o)
        self.last_op_per_sem[k] = o
        return o

    def _dma_key(self, eng):
        n = {"sp": 24, "pool": 12}.get(eng, 4)
        c = self.dma_cnt.get(eng, 0)
        self.dma_cnt[eng] = c + 1
        return ("dma", eng, c % n)

    def barrier(self):
        lasts = list(self.last_op_per_sem.values())
        for e in ENGS:
            o = Op(e, None)
            o.deps = list(lasts)
            for d in lasts:
                d.needed = True
            self.ops[e].append(o)
            self.all_ops.append(o)

    def final_wait(self, eng="sp"):
        lasts = list(self.last_op_per_sem.values())
        o = Op(eng, None)
        o.deps = lasts
        for d in lasts:
            d.needed = True
        self.ops[eng].append(o)
        self.all_ops.append(o)

    def emit(self):
        nc = self.nc
        counts = {}
        for o in self.all_ops:
            if o.fn is None:
                continue
            if o.needed:
                counts[o.semkey] = counts.get(o.semkey, 0) + o.inc
                o.ticket = counts[o.semkey]
        self.max_counts = counts
        with contextlib.ExitStack() as st:
            sems = {}
            for k in counts:
                nm = k if isinstance(k, str) else f"dma_{k[1]}{k[2]}"
                sems[k] = st.enter_context(nc.semaphore(f"s_{nm}"))
            block = st.enter_context(nc.Block())
            engmap = {"pe": block.tensor, "act": block.scalar, "dve": block.vector,
                      "pool": block.gpsimd, "sp": block.sync}

            def make(ename):
                oplist = self.ops[ename]

                def body(e):
                    waited = {}
                    for o in oplist:
                        need = {}
                        for d in o.deps:
                            if d.ticket is None:
                                continue
                            if need.get(d.semkey, 0) < d.ticket:
                                need[d.semkey] = d.ticket
                        for k, t in need.items():
                            if waited.get(k, 0) < t:
                                e.wait_ge(sems[k], t)
                                waited[k] = t
                        if o.fn is None:
                            continue
                        ins = o.fn(e)
                        if o.needed:
                            ins.then_inc(sems[o.semkey], o.inc)
                return body

            for ename in ENGS:
                if self.ops[ename]:
                    engmap[ename](make(ename))


class Arena:
    def __init__(self, nc, name, nwords):
        self.t = nc.alloc_sbuf_tensor(name, [128, nwords], F32)
        self.n = nwords
        self.off = 0
        self.marks = []

    def mark(self):
        self.marks.append(self.off)

    def release(self):
        self.off = self.marks.pop()

    def f32(self, n):
        n = (n + 1) // 2 * 2
        assert self.off + n <= self.n, f"arena overflow {self.off}+{n}>{self.n}"
        ap = self.t[:, self.off:self.off + n]
        self.off += n
        return ap

    def bf16(self, n):
        w = (n + 3) // 4 * 2
        return self.f32(w).bitcast(BF16)[:, 0:n]


def build(S, dbg=False, stop_after=None, n_exp=NE):
    NT = S // TT
    NS = S // 128
    nc = bass.Bass("TRN2", target_bir_lowering=False)

    def din(name, shape, dt=F32):
        return nc.dram_tensor(name, list(shape), dt, kind="ExternalInput").ap()

    x = din("x", [S, D])
    ln_mix_even = din("ln_mix_even", [D]); ln_ffn_even = din("ln_ffn_even", [D])
    ln_mix_odd = din("ln_mix_odd", [D]); ln_ffn_odd = din("ln_ffn_odd", [D])
    final_g = din("final_g", [D]); pool_scale = din("pool_scale", [D])
    w_in = din("w_in_even", [D, IN_W]); w_out = din("w_out_even", [D, D])
    lam_q1 = din("lam_q1", [64]); lam_k1 = din("lam_k1", [64])
    lam_q2 = din("lam_q2", [64]); lam_k2 = din("lam_k2", [64])
    subln_g = din("subln_g", [128])
    conv_w = din("conv_w", [4, 512]); conv_b = din("conv_b", [512])
    rg_wa = din("rg_wa", [8, 64, 64]); rg_ba = din("rg_ba", [512])
    rg_wx = din("rg_wx", [8, 64, 64]); rg_bx = din("rg_bx", [512])
    rg_lam = din("rg_lam", [512])
    ffn_wg = din("ffn_wg", [D, FFN]); ffn_wu = din("ffn_wu", [D, FFN]); ffn_wd = din("ffn_wd", [FFN, D])
    pool_w = din("pool_w", [4, 256, 256])
    router_w = din("router_w", [D, NE])
    moe_wg = din("moe_wg", [NE, D, EXP]); moe_wu = din("moe_wu", [NE, D, EXP]); moe_wd = din("moe_wd", [NE, EXP, D])
    ropet = din("ropet", [4, 128, S])
    invcnt = din("invcnt", [128, 8, 16])
    y = nc.dram_tensor("y", [S, D], F32, kind="ExternalOutput").ap()
    h1d = nc.dram_tensor("h1d", [S, D], F32, kind="ExternalOutput" if dbg else "Internal").ap()

    P = Prog(nc)
    A = Arena(nc, "arena", 52224)
    psum = [nc.alloc_psum_tensor(f"ps{i}", [128, 512], F32)[:, :] for i in range(8)]
    pbuf = [Buf(f"ps{i}") for i in range(8)]
    ybuf = Buf("y")
    h1buf = [Buf(f"h1_{i}") for i in range(NS)]

    def dump(name, ap, bufs, dt=BF16):
        if not dbg:
            return
        t = nc.dram_tensor(name, list(ap.shape), dt, kind="ExternalOutput").ap()
        P.dma("sp", t, ap, reads=list(bufs), writes=[Buf()])

    ident = A.bf16(128); b_ident = Buf()
    identf = A.f32(128); b_identf = Buf()
    ones_bf = A.bf16(128); b_ones = Buf()
    cst = A.f32(8); b_cst = Buf()
    P.op("pool", lambda e: e.memset(ident, 1.0), writes=[b_ident])
    P.op("pool", lambda e: e.affine_select(out=ident, in_=ident, pattern=[[-1, 128]], compare_op=ALU.is_equal,
                                           fill=0.0, base=0, channel_multiplier=1), reads=[b_ident], writes=[b_ident])
    P.op("pool", lambda e: e.memset(identf, 1.0), writes=[b_identf])
    P.op("pool", lambda e: e.affine_select(out=identf, in_=identf, pattern=[[-1, 128]], compare_op=ALU.is_equal,
                                           fill=0.0, base=0, channel_multiplier=1), reads=[b_identf], writes=[b_identf])
    P.op("pool", lambda e: e.memset(ones_bf, 1.0), writes=[b_ones])
    tri01 = A.bf16(128); b_tri = Buf()
    P.op("pool", lambda e: e.memset(tri01, 1.0), writes=[b_tri])
    P.op("pool", lambda e: e.affine_select(out=tri01, in_=tri01, pattern=[[1, 128]], compare_op=ALU.is_ge, fill=0.0, base=0, channel_multiplier=-1),
         reads=[b_tri], writes=[b_tri])
    P.op("pool", lambda e: e.memset(cst[:, 0:1], -0.5), writes=[b_cst])
    P.op("pool", lambda e: e.memset(cst[:, 1:2], NORM_EPS), writes=[b_cst])
    P.op("pool", lambda e: e.memset(cst[:, 2:3], SUBLN_EPS), writes=[b_cst])
    P.op("pool", lambda e: e.memset(cst[:, 3:4], 1.0), writes=[b_cst])

    def load_bcast(vec, n, name):
        t = A.f32(n); b = Buf(name)
        P.dma("sp", t, vec.partition_broadcast(128), writes=[b])
        return t, b

    def dscr(name, shape, dt=BF16):
        return nc.dram_tensor(name, list(shape), dt, kind="Internal").ap()

    w_in_b = dscr("w_in_b", [D, IN_W]); w_out_b = dscr("w_out_b", [D, D]); pool_w_b = dscr("pool_w_b", [4, 256, 256])
    NFG = FFN // 256
    ffn_gq = dscr("ffn_gq", [NFG, 128, KC * 256]); ffn_uq = dscr("ffn_uq", [NFG, 128, KC * 256]); ffn_dq = dscr("ffn_dq", [NFG, 128, 2 * D])
    QWc = EXP // 4
    moe_gq = [dscr(f"moe_gq{q}", [NE * 128, KC * QWc]) for q in range(4)]
    moe_uq = [dscr(f"moe_uq{q}", [NE * 128, KC * QWc]) for q in range(4)]
    moe_dq = [dscr(f"moe_dq{q}", [NE * 128, 7 * D]) for q in range(4)]
    xnTd = dscr("xnTd", [NT, 128, KC * TT]); b_xnTd = [Buf() for _ in range(NT)]
    TS = 384
    NSLOT_T = (2 * S + NE * (TS - 1)) // TS
    NSLOT = NSLOT_T * TS
    Xg = dscr("Xg", [NSLOT, D]); b_Xg = Buf()
    pc_q = []
    b_winb = [Buf() for _ in range(KC)]; b_woutb = [Buf() for _ in range(KC)]; b_pwb = [Buf() for _ in range(4)]
    b_fg = [Buf() for _ in range(FFN // 256)]; b_fu = [Buf() for _ in range(FFN // 256)]; b_fd = [Buf() for _ in range(FFN // 256)]
    b_moe = []

    def pc(dst, src, buf, now=False):
        if now:
            P.dma("pool", dst, src, writes=[buf])
        else:
            pc_q.append((dst, src, buf))

    def tick(n=1):
        for _ in range(n):
            if pc_q:
                dst, src, buf = pc_q.pop(0)
                P.dma("pool", dst, src, writes=[buf])

    for kc in range(KC):
        pc(w_in_b[kc * 128:(kc + 1) * 128, :], w_in[kc * 128:(kc + 1) * 128, :], b_winb[kc], now=True)
    for kc in range(KC):
        pc(w_out_b[kc * 128:(kc + 1) * 128, :], w_out[kc * 128:(kc + 1) * 128, :], b_woutb[kc])
    for g in range(4):
        pc(pool_w_b[g], pool_w[g], b_pwb[g])
    for g in range(FFN // 256):
        pc(ffn_gq[g].rearrange("p (k n) -> p k n", k=KC), ffn_wg.rearrange("(k p) n -> p k n", p=128)[:, :, g * 256:(g + 1) * 256], b_fg[g])
        pc(ffn_uq[g].rearrange("p (k n) -> p k n", k=KC), ffn_wu.rearrange("(k p) n -> p k n", p=128)[:, :, g * 256:(g + 1) * 256], b_fu[g])
        pc(ffn_dq[g].rearrange("p (c n) -> p c n", c=2), ffn_wd[g * 256:(g + 1) * 256, :].rearrange("(c p) n -> p c n", p=128), b_fd[g])
    for ex in range(NE):
        for q in range(4):
            for (dst, src) in ((moe_gq, moe_wg), (moe_uq, moe_wu)):
                b = Buf(); b_moe.append(b)
                pc(dst[q][ex * 128:(ex + 1) * 128, :].rearrange("p (k n) -> p k n", k=KC),
                   src[ex].rearrange("(k p) n -> p k n", p=128)[:, :, q * QWc:(q + 1) * QWc], b)
            b = Buf(); b_moe.append(b)
            pc(moe_dq[q][ex * 128:(ex + 1) * 128, :].rearrange("p (c n) -> p c n", c=7),
               moe_wd[ex, q * 7 * 128:(q + 1) * 7 * 128, :].rearrange("(c p) n -> p c n", p=128), b)

    def norm_T(src, b_src, g_bc, b_g, dstT, b_dstT, col0, tmp, ps_i, f32T=None, split=False):
        if isinstance(tmp, list):
            tmp = tmp[nrr[0] % len(tmp)]; nrr[0] += 1
        junk, ss, rstd, xn, b_tmp = tmp
        P.op("act", lambda e: e.activation(out=junk, in_=src, func=AF.Square, accum_out=ss), reads=[b_src], writes=[b_tmp[0]])
        P.op("dve", lambda e: e.tensor_scalar(out=rstd, in0=ss, scalar1=1.0 / D, scalar2=NORM_EPS, op0=ALU.mult, op1=ALU.add),
             reads=[b_tmp[0]], writes=[b_tmp[1]])
        P.op("pool", lambda e: e.tensor_tensor(out=rstd, in0=rstd, in1=cst[:, 0:1], op=ALU.pow), reads=[b_tmp[1], b_cst], writes=[b_tmp[1]])
        tick()
        if f32T is None:
            P.op("dve", lambda e: e.scalar_tensor_tensor(out=xn, in0=src, scalar=rstd, in1=g_bc, op0=ALU.mult, op1=ALU.mult),
                 reads=[b_src, b_tmp[1], b_g], writes=[b_tmp[2]])
        else:
            xT32, b_xT32, xn32, b_xn32, psi32 = f32T
            P.op("dve", lambda e: e.scalar_tensor_tensor(out=xn32, in0=src, scalar=rstd, in1=g_bc, op0=ALU.mult, op1=ALU.mult),
                 reads=[b_src, b_tmp[1], b_g], writes=[b_xn32])
            P.op("act", lambda e: e.activation(out=xn, in_=xn32, func=AF.Copy), reads=[b_xn32], writes=[b_tmp[2]])
            for hh in range(2):
                pp = psum[psi32[hh]]
                for c4 in range(4):
                    c = hh * 4 + c4
                    P.op("pe", lambda e, c=c, c4=c4, pp=pp: e.transpose(out=pp[:, c4 * 128:(c4 + 1) * 128], in_=xn32[:, c * 128:(c + 1) * 128], identity=identf),
                         reads=[b_xn32, b_identf], writes=[pbuf[psi32[hh]]])
                P.op("dve", lambda e, hh=hh, pp=pp: e.tensor_copy(out=xT32[:, hh * 4:(hh + 1) * 4, :], in_=pp.rearrange("p (c t) -> p c t", c=4)),
                     reads=[pbuf[psi32[hh]]], writes=[b_xT32])
        def part_b():
            pb = psum[ps_i].bitcast(BF16)
            for c in range(KC):
                P.op("pe", lambda e, c=c: e.transpose(out=pb[:, c * 128:(c + 1) * 128], in_=xn[:, c * 128:(c + 1) * 128], identity=ident),
                     reads=[b_tmp[2], b_ident], writes=[pbuf[ps_i]])
            P.op("act", lambda e: e.activation(out=dstT[:, :, col0:col0 + 128], in_=pb.rearrange("p (c t) -> p c t", c=KC), func=AF.Copy),
                 reads=[pbuf[ps_i]], writes=[b_dstT])
        if split:
            return part_b
        part_b()

    nrr = [0]

    def norm_tmp1():
        junk = A.bf16(D); ss = A.f32(2); rstd = A.f32(2); xn = A.bf16(D)
        return (junk, ss[:, 0:1], rstd[:, 0:1], xn, [Buf(), Buf(), Buf()])

    def norm_tmp(n=1):
        if n == 1:
            return norm_tmp1()
        sets = [norm_tmp1()]
        for _ in range(n - 1):
            ss = A.f32(2); rstd = A.f32(2); xn = A.bf16(D)
            sets.append((sets[0][0], ss[:, 0:1], rstd[:, 0:1], xn, [sets[0][4][0], Buf(), Buf()]))
        return sets

    A.mark()
    g0_bc, b_g0 = load_bcast(ln_mix_even, D, "g0")
    qT = A.bf16(4 * S).rearrange("p (c t) -> p c t", c=4); b_qT = [Buf() for _ in range(NT)]
    attnT = qT; b_attnT = b_qT
    ps_rr = [0]

    def next_ps(lo, hi):
        i = lo + ps_rr[0] % (hi - lo)
        ps_rr[0] += 1
        return i

    def in_proj_fm(wt, b_wt, col0, ps_i, xnT, b_xnT, kcn=KC):
        pp = psum[ps_i]
        for kc in range(kcn):
            P.op("pe", lambda e, kc=kc: e.matmul(pp, lhsT=wt[:, kc, col0:col0 + 128], rhs=xnT[:, kc, :], start=(kc == 0), stop=(kc == kcn - 1)),
                 reads=[b_wt, b_xnT], writes=[pbuf[ps_i]])

    tick(12)
    A.mark()
    kT = A.bf16(4 * S).rearrange("p (c t) -> p c t", c=4); b_kT = [Buf() for _ in range(NT)]
    Vt = A.bf16(NS * 512).rearrange("p (s n) -> p s n", s=NS); b_V = [Buf() for _ in range(NT)]
    A.mark()
    w_qkv = A.bf16(KC * 1536).rearrange("p (k n) -> p k n", k=KC); b_wqkv = Buf()
    for kc in range(KC):
        P.dma("sp", w_qkv[:, kc, :], w_in_b[kc * 128:(kc + 1) * 128, 0:1536], reads=[b_winb[kc]], writes=[b_wqkv])
    wsw = A.bf16(KC * 1024).rearrange("p (k n) -> p k n", k=KC); b_wsw = Buf()
    P.op("pool", lambda e: e.memset(wsw.rearrange("p k n -> p (k n)"), 0.0), writes=[b_wsw])
    srcv = w_qkv[:, :, 0:1024].rearrange("p k (g d) -> p k g d", d=64)
    dstv = wsw.rearrange("p k (g d) -> p k g d", d=64)
    P.op("dve", lambda e: e.tensor_copy(out=dstv[:, :, :, 0:8], in_=srcv[:, :, :, 8:16]), reads=[b_wqkv, b_wsw], writes=[b_wsw])
    P.op("dve", lambda e: e.tensor_copy(out=dstv[:, :, :, 8:16], in_=srcv[:, :, :, 0:8]), reads=[b_wqkv, b_wsw], writes=[b_wsw])
    zt = A.bf16(D); b_zt = Buf()
    P.op("dve", lambda e: e.memset(zt, 0.0), writes=[b_zt])
    zrows = list(range(0, NSLOT, 128))
    xt = [A.f32(D) for _ in range(2)]; b_xt = [Buf(), Buf()]
    ntmp = norm_tmp(2)
    xnTq = [A.bf16(KC * TT).rearrange("p (c t) -> p c t", c=KC) for _ in range(2)]; b_xnTq = [Buf(), Buf()]
    rtab = [A.f32(4 * TT).rearrange("p (f t) -> p f t", f=4) for _ in range(2)]; b_rtab = [Buf(), Buf()]
    t1 = [A.f32(TT) for _ in range(2)]; t2 = [A.f32(TT) for _ in range(2)]
    b_t1 = [Buf(), Buf()]; b_t2 = [Buf(), Buf()]

    def prep_q(tt):
        pend = []
        for s in range(4):
            tok0 = tt * TT + s * 128
            xi = (tt * 4 + s) % 2
            P.dma("sp", xt[xi], x[tok0:tok0 + 128, :], writes=[b_xt[xi]])
            pend.append(norm_T(xt[xi], b_xt[xi], g0_bc, b_g0, xnTq[tt % 2], b_xnTq[tt % 2], s * 128, ntmp, 7, split=True))
            yield
            if len(pend) > 1:
                pend.pop(0)()
            yield
        while pend:
            yield
            pend.pop(0)()
        P.dma("sp", xnTd[tt], xnTq[tt % 2].rearrange("p c t -> p (c t)"), reads=[b_xnTq[tt % 2]], writes=[b_xnTd[tt]])
        yield

    for _ in prep_q(0):
        pass
    for tt in range(NT):
        ri = tt % 2
        xnT = xnTq[tt % 2]; b_xnT = b_xnTq[tt % 2]
        genq = [prep_q(tt + 1) if tt + 1 < NT else None]

        def advq(n, genq=genq):
            for _ in range(n):
                if genq[0] is None:
                    return
                try:
                    next(genq[0])
                except StopIteration:
                    genq[0] = None

        P.dma("sp", rtab[ri], ropet[:, :, tt * TT:(tt + 1) * TT].rearrange("f p t -> p f t"), writes=[b_rtab[ri]])
        for _ in range((len(zrows) + NT - 1 - tt) // (NT - tt) if tt < NT - 1 else len(zrows)):
            if zrows:
                r0 = zrows.pop(0)
                P.dma("sp", Xg[r0:r0 + 128, :], zt, reads=[b_zt], writes=[Buf()])
        for hc in range(8):
            p1 = next_ps(0, 6);
            in_proj_fm(w_qkv, b_wqkv, hc * 128, p1, xnT, b_xnT)
            p2 = next_ps(0, 6)
            in_proj_fm(wsw, b_wsw, hc * 128, p2, xnT, b_xnT)
            isk = hc >= 4
            q = hc % 2
            P.op("dve", lambda e, q=q, p1=p1, ri=ri, isk=isk: e.tensor_tensor(out=t1[q], in0=psum[p1], in1=rtab[ri][:, 2 if isk else 0, :], op=ALU.mult),
                 reads=[pbuf[p1], b_rtab[ri]], writes=[b_t1[q]])
            P.op("dve", lambda e, q=q, p2=p2, ri=ri, isk=isk: e.tensor_tensor(out=t2[q], in0=psum[p2], in1=rtab[ri][:, 3 if isk else 1, :], op=ALU.mult),
                 reads=[pbuf[p2], b_rtab[ri]], writes=[b_t2[q]])
            dst = (kT if isk else qT)[:, hc % 4, tt * TT:(tt + 1) * TT]
            bd = (b_kT if isk else b_qT)[tt]
            P.op("pool", lambda e, q=q, dst=dst: e.tensor_tensor(out=dst, in0=t1[q], in1=t2[q], op=ALU.add), reads=[b_t1[q], b_t2[q]], writes=[bd])
            advq(1)
        for s in range(4):
            pv = next_ps(0, 6)
            for kc in range(KC):
                P.op("pe", lambda e, kc=kc, s=s, pv=pv: e.matmul(psum[pv], lhsT=xnT[:, kc, s * 128:(s + 1) * 128], rhs=w_qkv[:, kc, 1024:1536], start=(kc == 0), stop=(kc == KC - 1)),
                     reads=[b_xnT, b_wqkv], writes=[pbuf[pv]])
            P.op("act", lambda e, s=s, pv=pv, tt=tt: e.activation(out=Vt[:, tt * 4 + s, :], in_=psum[pv], func=AF.Copy), reads=[pbuf[pv]], writes=[b_V[tt]])
            advq(1)
        advq(10 ** 6)
    P.barrier()
    A.release()

    dump("d_qT", qT, b_qT); dump("d_kT", kT, b_kT); dump("d_V", Vt, b_V)
    P.barrier()
    A.mark()
    lv = [load_bcast(v_, 64, "lv")[0:2] for v_ in (lam_q1, lam_k1, lam_q2, lam_k2)]
    lsum = A.f32(4); b_ls = Buf(); ljunk = A.f32(64)
    neglam = A.f32(2); gsc = A.f32(2); b_nl = Buf()
    sg_t, b_sg = A.f32(2), Buf()
    P.dma("sp", sg_t[:, 0:1], subln_g.rearrange("(p o) -> p o", o=1), writes=[b_sg])
    P.op("dve", lambda e: e.tensor_tensor(out=ljunk, in0=lv[0][0], in1=lv[1][0], op=ALU.mult), reads=[lv[0][1], lv[1][1]], writes=[b_ls])
    P.op("dve", lambda e: e.tensor_reduce(out=lsum[:, 0:1], in_=ljunk, axis=mybir.AxisListType.X, op=ALU.add), reads=[b_ls], writes=[b_ls])
    P.op("dve", lambda e: e.tensor_tensor(out=ljunk, in0=lv[2][0], in1=lv[3][0], op=ALU.mult), reads=[lv[2][1], lv[3][1], b_ls], writes=[b_ls])
    P.op("dve", lambda e: e.tensor_reduce(out=lsum[:, 1:2], in_=ljunk, axis=mybir.AxisListType.X, op=ALU.add), reads=[b_ls], writes=[b_ls])
    P.op("act", lambda e: e.activation(out=lsum[:, 0:2], in_=lsum[:, 0:2], func=AF.Exp), reads=[b_ls], writes=[b_ls])
    P.op("dve", lambda e: e.tensor_tensor(out=neglam[:, 0:1], in0=lsum[:, 1:2], in1=lsum[:, 0:1], op=ALU.subtract), reads=[b_ls], writes=[b_nl])
    P.op("dve", lambda e: e.tensor_scalar(out=neglam[:, 0:1], in0=neglam[:, 0:1], scalar1=-LAM_INIT0, scalar2=None, op0=ALU.add), reads=[b_nl], writes=[b_nl])
    P.op("dve", lambda e: e.tensor_scalar(out=gsc[:, 0:1], in0=sg_t[:, 0:1], scalar1=(1.0 - LAM_INIT0), scalar2=None, op0=ALU.mult), reads=[b_sg], writes=[b_nl])
    ET = [[A.bf16(TT) for _ in range(2)] for _ in range(2)]; b_ET = [[Buf() for _ in range(2)] for _ in range(2)]
    r1 = A.f32(TT); r2 = A.f32(TT); o1 = A.f32(TT); osq = A.bf16(TT); b_ep = [Buf() for _ in range(4)]
    pend_epi = [None]
    for h in range(4):
        for qb in range(NT):
            q0 = qb * TT
            nkb = 4 * qb + 4
            def emit_S(kb):
                off = max(0, (kb - 4 * qb) * 128)
                n = TT - off
                eb = kb % 2
                for m in range(2):
                    ps_s = m * 2 + eb
                    P.op("pe", lambda e, m=m, kb=kb, off=off, ps_s=ps_s, n=n: e.matmul(
                        psum[ps_s][:, 0:n], lhsT=kT[m * 64:(m + 1) * 64, h, kb * 128:(kb + 1) * 128],
                        rhs=qT[m * 64:(m + 1) * 64, h, q0 + off:q0 + TT], start=True, stop=True),
                        reads=[b_kT[kb // 4], b_qT[qb]], writes=[pbuf[ps_s]])
                    P.op("act", lambda e, m=m, eb=eb, ps_s=ps_s, n=n: e.activation(out=ET[m][eb][:, 0:n], in_=psum[ps_s][:, 0:n], func=AF.Exp),
                         reads=[pbuf[ps_s]], writes=[b_ET[m][eb]])
                    if kb >= 4 * qb:
                        P.op("dve", lambda e, m=m, eb=eb: e.tensor_tensor(out=ET[m][eb][:, 0:128], in0=ET[m][eb][:, 0:128], in1=tri01, op=ALU.mult),
                             reads=[b_ET[m][eb], b_tri], writes=[b_ET[m][eb]])

            def emit_PV(kb):
                off = max(0, (kb - 4 * qb) * 128)
                n = TT - off
                eb = kb % 2
                for m in range(2):
                    P.op("pe", lambda e, m=m, eb=eb, kb=kb, off=off, n=n: e.matmul(
                        psum[4 + m][:, off:TT], lhsT=Vt[:, kb, h * 128:(h + 1) * 128], rhs=ET[m][eb][:, 0:n], start=(kb == 0), stop=(kb == nkb - 1)),
                        reads=[b_V[kb // 4], b_ET[m][eb]], writes=[pbuf[4 + m]])
                    P.op("pe", lambda e, m=m, eb=eb, kb=kb, off=off, n=n: e.matmul(
                        psum[6 + m][:, off:TT], lhsT=ones_bf, rhs=ET[m][eb][:, 0:n], start=(kb == 0), stop=(kb == nkb - 1)),
                        reads=[b_ones, b_ET[m][eb]], writes=[pbuf[6 + m]])

            emit_S(0)
            for kb in range(nkb):
                if kb + 1 < nkb:
                    emit_S(kb + 1)
                emit_PV(kb)
                if kb == 1 and pend_epi[0] is not None:
                    pend_epi[0]()
                    pend_epi[0] = None
            tick(3)
            P.op("dve", lambda e: e.reciprocal(out=r1, in_=psum[6]), reads=[pbuf[6]], writes=[b_ep[0]])
            P.op("dve", lambda e: e.reciprocal(out=r2, in_=psum[7]), reads=[pbuf[7]], writes=[b_ep[1]])
            P.op("dve", lambda e: e.tensor_tensor(out=r1, in0=psum[4], in1=r1, op=ALU.mult), reads=[pbuf[4], b_ep[0]], writes=[b_ep[0]])
            P.op("dve", lambda e: e.tensor_tensor(out=r2, in0=psum[5], in1=r2, op=ALU.mult), reads=[pbuf[5], b_ep[1]], writes=[b_ep[1]])
            P.op("dve", lambda e: e.scalar_tensor_tensor(out=o1, in0=r2, scalar=neglam[:, 0:1], in1=r1, op0=ALU.mult, op1=ALU.add),
                 reads=[b_ep[0], b_ep[1], b_nl], writes=[b_ep[2]])
            def epi_tail(h=h, q0=q0, qb=qb):
                P.op("act", lambda e: e.activation(out=osq, in_=o1, func=AF.Square), reads=[b_ep[2]], writes=[b_ep[3]])
                P.op("pe", lambda e: e.matmul(psum[1], lhsT=ones_bf, rhs=osq, start=True, stop=True), reads=[b_ones, b_ep[3]], writes=[pbuf[1]])
                P.op("act", lambda e: e.activation(out=r1, in_=psum[1], func=AF.Ln, scale=1.0 / 128, bias=cst[:, 2:3]), reads=[pbuf[1], b_cst, b_ep[0]], writes=[b_ep[0]])
                P.op("act", lambda e: e.activation(out=r1, in_=r1, func=AF.Exp, scale=-0.5), reads=[b_ep[0]], writes=[b_ep[0]])
                P.op("dve", lambda e: e.scalar_tensor_tensor(out=attnT[:, h, q0:q0 + TT], in0=o1, scalar=gsc[:, 0:1], in1=r1, op0=ALU.mult, op1=ALU.mult),
                     reads=[b_ep[2], b_ep[0], b_nl], writes=[b_attnT[qb]])
            pend_epi[0] = epi_tail
    pend_epi[0]()
    P.barrier()
    A.release()
    A.release()

    recT = A.bf16(4 * S).rearrange("p (c t) -> p c t", c=4); b_recT = [Buf() for _ in range(NT)]
    A.mark()
    w_rec = A.bf16(KC * 1024).rearrange("p (k n) -> p k n", k=KC); b_wrec = Buf()
    for kc in range(KC):
        P.dma("sp", w_rec[:, kc, :], w_in_b[kc * 128:(kc + 1) * 128, 1536:2560], reads=[b_winb[kc]], writes=[b_wrec])
    def load_cm(vec, name):
        t = A.f32(4); b = Buf(name)
        P.dma("sp", t, vec.rearrange("(c p) -> p c", p=128), writes=[b], allow_slow_non_contiguous=True)
        return t, b
    cw = A.f32(16).rearrange("p (j c) -> p j c", j=4); b_cw = Buf()
    P.dma("sp", cw, conv_w.rearrange("j (c p) -> p j c", p=128), writes=[b_cw], allow_slow_non_contiguous=True)
    cb, b_cb = load_cm(conv_b, "cb")
    ba_t, b_ba = load_cm(rg_ba, "ba")
    bx_t, b_bx = load_cm(rg_bx, "bx")
    lam_t, b_lam = load_cm(rg_lam, "lam")
    cneg = A.f32(4); c2 = A.f32(4); b_cn = Buf()
    P.op("act", lambda e: e.activation(out=cneg, in_=lam_t, func=AF.Exp, scale=-1.0), reads=[b_lam], writes=[b_cn])
    P.op("act", lambda e: e.activation(out=cneg, in_=cneg, func=AF.Ln, bias=cst[:, 3:4]), reads=[b_cn, b_cst], writes=[b_cn])
    P.op("dve", lambda e: e.tensor_scalar(out=c2, in0=cneg, scalar1=-16.0, scalar2=None, op0=ALU.mult), reads=[b_cn], writes=[b_cn])
    P.op("dve", lambda e: e.tensor_scalar(out=cneg, in0=cneg, scalar1=-8.0, scalar2=None, op0=ALU.mult), reads=[b_cn], writes=[b_cn])
    wbd_f = A.f32(2 * 4 * 128).rearrange("p (w c n) -> p w c n", w=2, c=4); b_wbdf = Buf()
    wbd = A.bf16(2 * 4 * 128).rearrange("p (w c n) -> p w c n", w=2, c=4); b_wbd = Buf()
    P.op("pool", lambda e: e.memset(wbd_f.rearrange("p w c n -> p (w c n)"), 0.0), writes=[b_wbdf])
    for wi, wsrc in enumerate((rg_wa, rg_wx)):
        for hl in range(2):
            P.dma("sp", wbd_f[hl * 64:(hl + 1) * 64, wi, :, hl * 64:(hl + 1) * 64],
                  wsrc.rearrange("(c h) i j -> h i c j", h=2)[hl], reads=[], writes=[b_wbdf])
    P.op("dve", lambda e: e.tensor_copy(out=wbd.rearrange("p w c n -> p (w c n)"), in_=wbd_f.rearrange("p w c n -> p (w c n)")),
         reads=[b_wbdf], writes=[b_wbd])

    xnT_r = [A.bf16(KC * TT).rearrange("p (c t) -> p c t", c=KC) for _ in range(2)]; b_xnT_r = [Buf(), Buf()]
    xbT = [A.f32(4 * (TT + 4)).rearrange("p (c t) -> p c t", c=4) for _ in range(2)]; b_xb = [Buf() for _ in range(2)]
    gbT = A.f32(4 * TT).rearrange("p (c t) -> p c t", c=4); b_gb = Buf()
    NTMP = 6
    rt = [[A.f32(TT) for _ in range(NTMP)] for _ in range(4)]
    b_rt = [[Buf() for _ in range(NTMP)] for _ in range(4)]
    xcb = [A.bf16(TT) for _ in range(4)]; b_xcb = [Buf() for _ in range(4)]
    hst = [A.f32(4 * TT).rearrange("p (c t) -> p c t", c=4) for _ in range(2)]; b_hst = [Buf() for _ in range(2)]
    P.op("pool", lambda e: e.memset(xbT[0][:, :, 0:4], 0.0), writes=[b_xb[0]])

    for tt in range(NT):
        cur = tt % 2; prv = 1 - cur
        xnT = xnT_r[cur]; b_xnT = b_xnT_r[cur]
        P.dma("sp", xnT.rearrange("p c t -> p (c t)"), xnTd[tt], reads=[b_xnTd[tt]], writes=[b_xnT])
        tick(4)
        for c in range(4):
            pi = next_ps(0, 4)
            in_proj_fm(w_rec, b_wrec, c * 128, pi, xnT, b_xnT)
            P.op("act", lambda e, c=c, pi=pi: e.activation(out=xbT[cur][:, c, 4:4 + TT], in_=psum[pi], func=AF.Copy), reads=[pbuf[pi]], writes=[b_xb[cur]])
        for c in range(4):
            pi = next_ps(0, 4)
            in_proj_fm(w_rec, b_wrec, 512 + c * 128, pi, xnT, b_xnT)
            P.op("act", lambda e, c=c, pi=pi: e.activation(out=gbT[:, c, :], in_=psum[pi], func=AF.Copy), reads=[pbuf[pi]], writes=[b_gb])
        if tt > 0:
            P.op("pool", lambda e, cur=cur, prv=prv: e.tensor_copy(out=xbT[cur][:, :, 1:4], in_=xbT[prv][:, :, TT + 1:TT + 4]),
                 reads=[b_xb[prv]], writes=[b_xb[cur]])
        XB = [xbT[cur][:, c, :] for c in range(4)]
        for c in range(4):
            xc = rt[c][0]; BT = b_rt[c]
            P.op("dve", lambda e, c=c, xc=xc: e.tensor_scalar(out=xc, in0=XB[c][:, 4:4 + TT], scalar1=cw[:, 3, c:c + 1], scalar2=cb[:, c:c + 1], op0=ALU.mult, op1=ALU.add),
                 reads=[b_xb[cur], b_cw, b_cb], writes=[BT[0]])
            for j in range(3):
                P.op("dve", lambda e, c=c, j=j, xc=xc: e.scalar_tensor_tensor(out=xc, in0=XB[c][:, 1 + j:1 + j + TT], scalar=cw[:, j, c:c + 1], in1=xc, op0=ALU.mult, op1=ALU.add),
                     reads=[b_xb[cur], b_cw, BT[0]], writes=[BT[0]])
            P.op("pool", lambda e, c=c, xc=xc: e.tensor_copy(out=xcb[c], in_=xc), reads=[BT[0]], writes=[b_xcb[c]])
        for c in range(4):
            gb_c = gbT[:, c, :]; sq = rt[c][5]; BT = b_rt[c]
            P.op("pool", lambda e, gb_c=gb_c, sq=sq: e.tensor_tensor(out=sq, in0=gb_c, in1=gb_c, op=ALU.mult), reads=[b_gb], writes=[BT[5]])
            P.op("pool", lambda e, sq=sq: e.tensor_scalar(out=sq, in0=sq, scalar1=0.044715, scalar2=1.0, op0=ALU.mult, op1=ALU.add), reads=[BT[5]], writes=[BT[5]])
            P.op("pool", lambda e, gb_c=gb_c, sq=sq: e.tensor_tensor(out=sq, in0=sq, in1=gb_c, op=ALU.mult), reads=[BT[5], b_gb], writes=[BT[5]])
        for c in range(4):
            r_ = rt[c][1]; i_ = rt[c][2]; BT = b_rt[c]
            pa = next_ps(4, 7)
            P.op("pe", lambda e, c=c, pa=pa: e.matmul(psum[pa], lhsT=wbd[:, 0, c, :], rhs=xcb[c], start=True, stop=True), reads=[b_wbd, b_xcb[c]], writes=[pbuf[pa]])
            P.op("act", lambda e, c=c, pa=pa, r_=r_: e.activation(out=r_, in_=psum[pa], func=AF.Sigmoid, bias=ba_t[:, c:c + 1]), reads=[pbuf[pa], b_ba], writes=[BT[1]])
            px = next_ps(4, 7)
            P.op("pe", lambda e, c=c, px=px: e.matmul(psum[px], lhsT=wbd[:, 1, c, :], rhs=xcb[c], start=True, stop=True), reads=[b_wbd, b_xcb[c]], writes=[pbuf[px]])
            P.op("act", lambda e, c=c, px=px, i_=i_: e.activation(out=i_, in_=psum[px], func=AF.Sigmoid, bias=bx_t[:, c:c + 1]), reads=[pbuf[px], b_bx], writes=[BT[2]])
        for c in range(4):
            sq = rt[c][5]; ge = rt[c][5]; BT = b_rt[c]; gb_c = gbT[:, c, :]
            P.op("act", lambda e, sq=sq, ge=ge: e.activation(out=ge, in_=sq, func=AF.Sigmoid, scale=1.5957691216057308), reads=[BT[5]], writes=[BT[5]])
            P.op("pool", lambda e, gb_c=gb_c, ge=ge: e.tensor_tensor(out=ge, in0=ge, in1=gb_c, op=ALU.mult), reads=[BT[5], b_gb], writes=[BT[5]])
        for c in range(4):
            r_ = rt[c][1]; a_ = rt[c][3]; a2 = rt[c][4]; BT = b_rt[c]
            P.op("act", lambda e, c=c, r_=r_, a_=a_: e.activation(out=a_, in_=r_, func=AF.Exp, scale=cneg[:, c:c + 1]), reads=[BT[1], b_cn], writes=[BT[3]])
            P.op("act", lambda e, c=c, r_=r_, a2=a2: e.activation(out=a2, in_=r_, func=AF.Exp, scale=c2[:, c:c + 1]), reads=[BT[1], b_cn], writes=[BT[4]])
            P.op("dve", lambda e, a2=a2: e.tensor_scalar(out=a2, in0=a2, scalar1=1.0, scalar2=0.0, op0=ALU.subtract, op1=ALU.min), reads=[BT[4]], writes=[BT[4]])
            xc = rt[c][0]; i_ = rt[c][2]; u_ = rt[c][2]
            P.op("dve", lambda e, i_=i_, xc=xc, u_=u_: e.tensor_tensor(out=u_, in0=i_, in1=xc, op=ALU.mult), reads=[BT[2], BT[0]], writes=[BT[2]])
        for c in range(4):
            a2 = rt[c][4]; BT = b_rt[c]
            P.op("act", lambda e, a2=a2: e.activation(out=a2, in_=a2, func=AF.Sqrt, scale=-1.0), reads=[BT[4]], writes=[BT[4]])
        for c in range(4):
            a_ = rt[c][3]; a2 = rt[c][4]; u_ = rt[c][2]; ge = rt[c][5]; BT = b_rt[c]
            P.op("dve", lambda e, a2=a2, u_=u_: e.tensor_tensor(out=u_, in0=u_, in1=a2, op=ALU.mult), reads=[BT[2], BT[4]], writes=[BT[2]])
            init = 0.0 if tt == 0 else hst[prv][:, c, TT - 1:TT]
            P.op("dve", lambda e, c=c, a_=a_, u_=u_, init=init: e.tensor_tensor_scan(out=hst[cur][:, c, :], data0=a_, data1=u_, initial=init, op0=ALU.mult, op1=ALU.add),
                 reads=[BT[3], BT[2], b_hst[prv]], writes=[b_hst[cur]])
            P.op("dve", lambda e, c=c, ge=ge: e.tensor_tensor(out=recT[:, c, tt * TT:(tt + 1) * TT], in0=hst[cur][:, c, :], in1=ge, op=ALU.mult),
                 reads=[b_hst[cur], BT[5]], writes=[b_recT[tt]])
    P.barrier()
    A.release()

    dump("d_attnT", attnT, b_attnT); dump("d_recT", recT, b_recT)
    P.barrier()
    A.mark()
    w_o = A.bf16(KC * D).rearrange("p (k n) -> p k n", k=KC); b_wo = Buf()
    for kc in range(KC):
        P.dma("sp", w_o[:, kc, :], w_out_b[kc * 128:(kc + 1) * 128, :], reads=[b_woutb[kc]], writes=[b_wo])
    xt = [A.f32(D) for _ in range(3)]; b_xt = [Buf() for _ in range(3)]
    for si in range(NS):
        tt = si // 4
        xi = si % 3
        P.dma("sp", xt[xi], x[si * 128:(si + 1) * 128, :], writes=[b_xt[xi]])
        for d in range(2):
            pi = next_ps(0, 8)
            for kc in range(KC):
                src = attnT[:, kc, si * 128:(si + 1) * 128] if kc < 4 else recT[:, kc - 4, si * 128:(si + 1) * 128]
                bsrc = b_attnT[tt] if kc < 4 else b_recT[tt]
                P.op("pe", lambda e, kc=kc, src=src, d=d, pi=pi: e.matmul(psum[pi], lhsT=src, rhs=w_o[:, kc, d * 512:(d + 1) * 512], start=(kc == 0), stop=(kc == KC - 1)),
                     reads=[bsrc, b_wo], writes=[pbuf[pi]])
            P.op("dve", lambda e, xi=xi, d=d, pi=pi: e.tensor_tensor(out=xt[xi][:, d * 512:(d + 1) * 512], in0=psum[pi], in1=xt[xi][:, d * 512:(d + 1) * 512], op=ALU.add),
                 reads=[pbuf[pi], b_xt[xi]], writes=[b_xt[xi]])
        P.dma("sp", h1d[si * 128:(si + 1) * 128, :], xt[xi], reads=[b_xt[xi]], writes=[h1buf[si]])
    P.barrier()
    A.release()
    A.release()

    if stop_after == "A":
        P.final_wait("sp")
        P.emit()
        return nc

    NJ = (S + TS - 1) // TS
    h3d = dscr("h3d", [S, D], F32); b_h3 = [Buf() for _ in range(NS)]
    xnd = dscr("xnd", [S, D]); b_xnd = [Buf() for _ in range(NS)]
    Yd = dscr("Yd", [NSLOT, D], F32); b_Yd = Buf()

    M1 = A.f32(NS * NE).rearrange("p (s n) -> p s n", s=NS); M2 = A.f32(NS * NE).rearrange("p (s n) -> p s n", s=NS)
    G = A.f32(NS * 2).rearrange("p (s n) -> p s n", s=NS); b_rt_info = Buf()
    sgt = [A.f32(TT) for _ in range(2)]; b_sgt = [Buf() for _ in range(2)]
    gu_rr = [0]

    def swiglu_core(nch, gs, wg_t, wu_t, b_wg, b_wu, dma_gu, dma_wd, nwd, xin, b_xin, hidT, b_hid, wd_of, sink, ntok=TT, hook=None):
        ngrp = nch // gs
        nslots = len(wg_t)
        pend_wd = list(range(nwd))
        for g in range(ngrp):
            sl = gu_rr[0] % nslots; gu_rr[0] += 1
            dma_gu(g, sl)
            if g >= 1 and pend_wd:
                for _ in range(max(1, (nwd + ngrp - 2) // max(1, ngrp - 1))):
                    if pend_wd:
                        dma_wd(pend_wd.pop(0))
            for cc in range(gs):
                c = gs * g + cc
                pg = (c % 2) * 2; pu = pg + 1
                for kc in range(KC):
                    P.op("pe", lambda e, kc=kc, cc=cc, sl=sl, pg=pg: e.matmul(psum[pg][:, 0:ntok], lhsT=wg_t[sl][:, kc, cc * 128:(cc + 1) * 128], rhs=xin[:, kc, :], start=(kc == 0), stop=(kc == KC - 1)),
                         reads=[b_wg[sl], b_xin], writes=[pbuf[pg]])
                for kc in range(KC):
                    P.op("pe", lambda e, kc=kc, cc=cc, sl=sl, pu=pu: e.matmul(psum[pu][:, 0:ntok], lhsT=wu_t[sl][:, kc, cc * 128:(cc + 1) * 128], rhs=xin[:, kc, :], start=(kc == 0), stop=(kc == KC - 1)),
                         reads=[b_wu[sl], b_xin], writes=[pbuf[pu]])
                q = c % 2
                P.op("act", lambda e, q=q, pg=pg: e.activation(out=sgt[q][:, 0:ntok], in_=psum[pg][:, 0:ntok], func=AF.Silu), reads=[pbuf[pg]], writes=[b_sgt[q]])
                P.op("dve", lambda e, q=q, pu=pu, c=c: e.tensor_tensor(out=hidT[:, c, :], in0=sgt[q][:, 0:ntok], in1=psum[pu][:, 0:ntok], op=ALU.mult), reads=[b_sgt[q], pbuf[pu]], writes=[b_hid[c]])
                if hook is not None:
                    hook()
        while pend_wd:
            dma_wd(pend_wd.pop(0))
        k = 0
        for s in range(ntok // 128):
            for d in range(2):
                pd = 4 + (k % 2); k += 1
                for c in range(nch):
                    wap, wbuf = wd_of(c)
                    P.op("pe", lambda e, c=c, s=s, d=d, pd=pd, wap=wap: e.matmul(psum[pd], lhsT=hidT[:, c, s * 128:(s + 1) * 128], rhs=wap[:, d * 512:(d + 1) * 512], start=(c == 0), stop=(c == nch - 1)),
                         reads=[b_hid[c], wbuf], writes=[pbuf[pd]])
                sink(s, d, pd)
                if hook is not None:
                    hook()

    tick(max(0, len(pc_q) - 96))
    A.mark()
    g1_bc, b_g1 = load_bcast(ln_ffn_even, D, "g1")
    g2_bc, b_g2 = load_bcast(ln_mix_odd, D, "g2")
    g3_bc, b_g3 = load_bcast(ln_ffn_odd, D, "g3")
    pw_s = A.bf16(4 * 2 * 256).rearrange("p (g k n) -> p g k n", g=4, k=2); b_pws = Buf()
    A.mark()
    psc_bc, b_psc = load_bcast(pool_scale, D, "psc")
    pw = A.bf16(4 * 2 * 256).rearrange("p (g k n) -> p g k n", g=4, k=2); b_pw = Buf()
    for g in range(4):
        P.dma("sp", pw[:, g, :, :], pool_w_b[g].rearrange("(k p) n -> p k n", p=128), reads=[b_pwb[g]], writes=[b_pw])
    for g in range(4):
        for k2 in range(2):
            P.op("dve", lambda e, g=g, k2=k2: e.tensor_tensor(out=pw_s[:, g, k2, :], in0=pw[:, g, k2, :], in1=psc_bc[:, g * 256:(g + 1) * 256], op=ALU.mult),
                 reads=[b_pw, b_psc], writes=[b_pws])
    P.barrier()
    A.release()
    rw = A.f32(KC * NE).rearrange("p (k n) -> p k n", k=KC); b_rw = Buf()
    P.dma("sp", rw, router_w.rearrange("(k p) n -> p k n", p=128), writes=[b_rw])
    icnt = A.f32(8 * 16).rearrange("p (c t) -> p c t", c=8); b_icnt = Buf()
    P.dma("sp", icnt, invcnt, writes=[b_icnt])

    R2 = [A.f32(4 * D).rearrange("p (s n) -> p s n", s=4) for _ in range(2)]; b_R2 = [[Buf() for _ in range(4)] for _ in range(2)]
    ntmp = norm_tmp(2)
    xnT2 = [A.bf16(KC * TT).rearrange("p (c t) -> p c t", c=KC) for _ in range(2)]; b_xnT2 = [Buf(), Buf()]
    HAL = 16
    xpT = A.bf16(KC * (TT + HAL)).rearrange("p (c t) -> p c t", c=KC); b_xpT = Buf()
    halo = A.bf16(KC * HAL).rearrange("p (c t) -> p c t", c=KC); b_halo = Buf()
    pacc_l = [A.f32(2 * (TT + HAL)).rearrange("p (c t) -> p c t", c=2)] * 2; b_pacc_l = [Buf()] * 2
    pacc2_l = [A.f32(2 * (TT + HAL)).rearrange("p (c t) -> p c t", c=2)] * 2; b_pacc2_l = [Buf()] * 2
    dT = A.bf16(KC * TT).rearrange("p (c t) -> p c t", c=KC); b_dT = Buf()
    xn32 = A.f32(D); b_xn32 = Buf()
    xnb = [A.bf16(D)] * 2; b_xnb = [Buf()] * 2
    xT32 = A.f32(KC * 128).rearrange("p (c t) -> p c t", c=KC); b_xT32 = Buf()
    lg = A.f32(8); b_lg = Buf()
    top8 = A.f32(8); b_top8 = Buf()
    gt = A.f32(2); b_m = Buf()
    NCF = FFN // 128
    NGU = 3
    wg_t = [A.bf16(KC * 256).rearrange("p (k n) -> p k n", k=KC) for _ in range(NGU)]; b_wg = [Buf() for _ in range(NGU)]
    wu_t = [A.bf16(KC * 256).rearrange("p (k n) -> p k n", k=KC) for _ in range(NGU)]; b_wu = [Buf() for _ in range(NGU)]
    hidF = A.bf16(NCF * TT).rearrange("p (c t) -> p c t", c=NCF); b_hidF = [Buf() for _ in range(NCF)]
    wdF = A.bf16(NCF * D).rearrange("p (c n) -> p c n", c=NCF); b_wdF = [Buf() for _ in range(NCF // 2)]
    P.op("pool", lambda e: e.memset(halo.rearrange("p c t -> p (c t)"), 0.0), writes=[b_halo])
    P.op("pool", lambda e: e.memset(pacc_l[0].rearrange("p c t -> p (c t)"), 0.0), writes=[b_pacc_l[0]])
    P.op("pool", lambda e: e.memset(pacc2_l[0].rearrange("p c t -> p (c t)"), 0.0), writes=[b_pacc2_l[0]])

    def ffn_gu(g, sl):
        P.dma("sp", wg_t[sl].rearrange("p k n -> p (k n)"), ffn_gq[g], reads=[b_fg[g]], writes=[b_wg[sl]])
        P.dma("sp", wu_t[sl].rearrange("p k n -> p (k n)"), ffn_uq[g], reads=[b_fu[g]], writes=[b_wu[sl]])

    def ffn_wd(g):
        P.dma("sp", wdF[:, 2 * g:2 * g + 2, :].rearrange("p c n -> p (c n)"), ffn_dq[g], reads=[b_fd[g]], writes=[b_wdF[g]])

    POOLW = (2, 4, 8, 16)
    W = TT + HAL

    def tail(tt):
        R = R2[tt % 2]; b_R = b_R2[tt % 2]
        P.op("pool", lambda e: e.tensor_copy(out=xpT[:, :, 0:HAL], in_=halo), reads=[b_halo], writes=[b_xpT])
        pend = []
        for s in range(4):
            pend.append(norm_T(R[:, s, :], b_R[s], g2_bc, b_g2, xpT, b_xpT, HAL + s * 128, ntmp, 7, split=True))
            yield
            if len(pend) > 1:
                pend.pop(0)()
            yield
        while pend:
            yield
            pend.pop(0)()
        yield
        P.op("pool", lambda e: e.tensor_copy(out=halo, in_=xpT[:, :, TT:TT + HAL]), reads=[b_xpT], writes=[b_halo])
        for g in range(4):
            cs = slice(2 * g, 2 * g + 2)
            nlev = g + 1
            pacc, b_pacc = pacc_l[g % 2], b_pacc_l[g % 2]
            pacc2, b_pacc2 = pacc2_l[g % 2], b_pacc2_l[g % 2]
            eng = "dve" if g % 2 == 0 else "pool"
            srcap = xpT[:, cs, :]
            bsrc = b_xpT
            for lv_ in range(nlev):
                sh = 1 << lv_
                dst, bdst = (pacc, b_pacc) if lv_ % 2 == 0 else (pacc2, b_pacc2)
                P.op(eng, lambda e, dst=dst, srcap=srcap, sh=sh: e.tensor_tensor(out=dst[:, :, sh:W], in0=srcap[:, :, sh:W], in1=srcap[:, :, 0:W - sh], op=ALU.add),
                     reads=[bsrc], writes=[bdst])
                srcap, bsrc = dst, bdst
                yield
            w = POOLW[g]
            P.op("dve", lambda e, srcap=srcap, cs=cs, w=w: e.scalar_tensor_tensor(out=dT[:, cs, :], in0=srcap[:, :, HAL:W], scalar=1.0 / w, in1=xpT[:, cs, HAL:W], op0=ALU.mult, op1=ALU.subtract),
                 reads=[bsrc, b_xpT], writes=[b_dT])
            if tt == 0:
                tmpap, btmp = (pacc2, b_pacc2) if srcap is pacc else (pacc, b_pacc)
                P.op("dve", lambda e, srcap=srcap, cs=cs, tmpap=tmpap: e.tensor_tensor(out=tmpap[:, :, 0:16], in0=srcap[:, :, HAL:HAL + 16], in1=icnt[:, cs, :], op=ALU.mult),
                     reads=[bsrc, b_icnt], writes=[btmp])
                P.op("dve", lambda e, tmpap=tmpap, cs=cs: e.tensor_tensor(out=dT[:, cs, 0:16], in0=tmpap[:, :, 0:16], in1=xpT[:, cs, HAL:HAL + 16], op=ALU.subtract),
                     reads=[btmp, b_xpT, b_dT], writes=[b_dT])
            yield
        for s in range(4):
            for gp in range(2):
                for g in (2 * gp, 2 * gp + 1):
                    for k2 in range(2):
                        P.op("pe", lambda e, s=s, g=g, k2=k2: e.matmul(psum[6][:, (g % 2) * 256:(g % 2) * 256 + 256], lhsT=dT[:, 2 * g + k2, s * 128:(s + 1) * 128], rhs=pw_s[:, g, k2, :], start=(k2 == 0), stop=(k2 == 1)),
                             reads=[b_dT, b_pws], writes=[pbuf[6]])
                P.op("dve", lambda e, s=s, gp=gp: e.tensor_tensor(out=R[:, s, gp * 512:(gp + 1) * 512], in0=psum[6], in1=R[:, s, gp * 512:(gp + 1) * 512], op=ALU.add),
                     reads=[pbuf[6], b_R[s]], writes=[b_R[s]])
                yield
        junk, ss, rstd, xn_unused, b_tmp = ntmp[0]
        for s in range(4):
            si = tt * 4 + s
            xi = si % 2
            P.dma("pool", h3d[si * 128:(si + 1) * 128, :], R[:, s, :], reads=[b_R[s]], writes=[b_h3[si]])
            P.op("act", lambda e, s=s: e.activation(out=junk, in_=R[:, s, :], func=AF.Square, accum_out=ss), reads=[b_R[s]], writes=[b_tmp[0]])
            P.op("dve", lambda e: e.tensor_scalar(out=rstd, in0=ss, scalar1=1.0 / D, scalar2=NORM_EPS, op0=ALU.mult, op1=ALU.add), reads=[b_tmp[0]], writes=[b_tmp[1]])
            P.op("pool", lambda e: e.tensor_tensor(out=rstd, in0=rstd, in1=cst[:, 0:1], op=ALU.pow), reads=[b_tmp[1], b_cst], writes=[b_tmp[1]])
            tick()
            P.op("dve", lambda e, s=s: e.scalar_tensor_tensor(out=xn32, in0=R[:, s, :], scalar=rstd, in1=g3_bc, op0=ALU.mult, op1=ALU.mult),
                 reads=[b_R[s], b_tmp[1], b_g3], writes=[b_xn32])
            P.op("act", lambda e, xi=xi: e.activation(out=xnb[xi], in_=xn32, func=AF.Copy), reads=[b_xn32], writes=[b_xnb[xi]])
            P.dma("pool", xnd[si * 128:(si + 1) * 128, :], xnb[xi], reads=[b_xnb[xi]], writes=[b_xnd[si]])
            yield
            yield
            for hh in range(2):
                pp = psum[6]
                for c4 in range(4):
                    c = hh * 4 + c4
                    P.op("pe", lambda e, c=c, c4=c4, pp=pp: e.transpose(out=pp[:, c4 * 128:(c4 + 1) * 128], in_=xn32[:, c * 128:(c + 1) * 128], identity=identf),
                         reads=[b_xn32, b_identf], writes=[pbuf[6]])
                P.op("dve", lambda e, hh=hh, pp=pp: e.tensor_copy(out=xT32[:, hh * 4:(hh + 1) * 4, :], in_=pp.rearrange("p (c t) -> p c t", c=4)),
                     reads=[pbuf[6]], writes=[b_xT32])
            yield
            for kc in range(KC):
                P.op("pe", lambda e, kc=kc: e.matmul(psum[6][:, 0:NE], lhsT=xT32[:, kc, :], rhs=rw[:, kc, :], start=(kc == 0), stop=(kc == KC - 1)),
                     reads=[b_xT32, b_rw], writes=[pbuf[6]])
            P.op("dve", lambda e: e.tensor_copy(out=lg, in_=psum[6][:, 0:NE]), reads=[pbuf[6]], writes=[b_lg])
            P.op("dve", lambda e: e.max(out=top8, in_=lg), reads=[b_lg], writes=[b_top8])
            P.op("dve", lambda e, si=si: e.tensor_scalar(out=M1[:, si, :], in0=lg, scalar1=top8[:, 0:1], scalar2=None, op0=ALU.is_equal), reads=[b_lg, b_top8], writes=[b_rt_info])
            P.op("dve", lambda e, si=si: e.tensor_scalar(out=M2[:, si, :], in0=lg, scalar1=top8[:, 1:2], scalar2=None, op0=ALU.is_equal), reads=[b_lg, b_top8], writes=[b_rt_info])
            P.op("dve", lambda e: e.tensor_tensor(out=gt[:, 0:1], in0=top8[:, 1:2], in1=top8[:, 0:1], op=ALU.subtract), reads=[b_top8], writes=[b_m])
            P.op("act", lambda e: e.activation(out=gt[:, 0:1], in_=gt[:, 0:1], func=AF.Exp), reads=[b_m], writes=[b_m])
            P.op("dve", lambda e: e.tensor_scalar(out=gt[:, 0:1], in0=gt[:, 0:1], scalar1=1.0, scalar2=None, op0=ALU.add), reads=[b_m], writes=[b_m])
            P.op("dve", lambda e, si=si: e.reciprocal(out=G[:, si, 0:1], in_=gt[:, 0:1]), reads=[b_m], writes=[b_rt_info])
            P.op("dve", lambda e, si=si: e.tensor_scalar(out=G[:, si, 1:2], in0=G[:, si, 0:1], scalar1=-1.0, scalar2=1.0, op0=ALU.mult, op1=ALU.add), reads=[b_rt_info], writes=[b_rt_info])
            yield

    def prep(tt):
        R = R2[tt % 2]; b_R = b_R2[tt % 2]
        pend = None
        for s in range(4):
            si = tt * 4 + s
            P.dma("pool", R[:, s, :], h1d[si * 128:(si + 1) * 128, :], reads=[h1buf[si]], writes=[b_R[s]])
        yield
        pend = []
        for s in range(4):
            pend.append(norm_T(R[:, s, :], b_R[s], g1_bc, b_g1, xnT2[tt % 2], b_xnT2[tt % 2], s * 128, ntmp, 7, split=True))
            yield
            if len(pend) > 1:
                pend.pop(0)()
            yield
        while pend:
            yield
            pend.pop(0)()
        yield

    def chain(gens):
        for g in gens:
            yield from g

    prev_tail = [None]

    def advance(n):
        g = prev_tail[0]
        if g is None:
            return
        for _ in range(n):
            try:
                next(g)
            except StopIteration:
                prev_tail[0] = None
                return

    for _ in prep(0):
        pass
    for tt in range(NT):
        R = R2[tt % 2]; b_R = b_R2[tt % 2]
        gens = []
        if tt > 0:
            gens.append(tail(tt - 1))
        if tt + 1 < NT:
            gens.append(prep(tt + 1))
        prev_tail[0] = chain(gens)

        def add_plain(s, d, pd, R=R, b_R=b_R):
            P.op("dve", lambda e: e.tensor_tensor(out=R[:, s, d * 512:(d + 1) * 512], in0=psum[pd], in1=R[:, s, d * 512:(d + 1) * 512], op=ALU.add),
                 reads=[pbuf[pd], b_R[s]], writes=[b_R[s]])

        swiglu_core(NCF, 2, wg_t, wu_t, b_wg, b_wu, ffn_gu, ffn_wd, NCF // 2, xnT2[tt % 2], b_xnT2[tt % 2], hidF, b_hidF, lambda c: (wdF[:, c, :], b_wdF[c // 2]), add_plain,
                    hook=lambda: advance(3))
        advance(10 ** 6)
    for _ in tail(NT - 1):
        pass
    tick(len(pc_q))
    P.barrier()
    A.release()

    A.mark()
    NSE = NS * NE
    triu = A.bf16(128); b_triu = Buf()
    P.op("pool", lambda e: e.memset(triu, 1.0), writes=[b_triu])
    P.op("pool", lambda e: e.affine_select(out=triu, in_=triu, pattern=[[1, 128]], compare_op=ALU.is_gt, fill=0.0, base=0, channel_multiplier=-1),
         reads=[b_triu], writes=[b_triu])
    thi = A.f32(NSLOT_T + 1).bitcast(I32); thj = A.f32(NSLOT_T + 1); zer = A.f32(max(NS, 8)); b_th = Buf()
    th8 = thj[:, 0:NJ]
    P.op("pool", lambda e: e.iota(thi[:, 0:NSLOT_T], pattern=[[TS, NSLOT_T]], base=0, channel_multiplier=0), writes=[b_th])
    P.op("dve", lambda e: e.tensor_copy(out=thj[:, 0:NSLOT_T], in_=thi[:, 0:NSLOT_T]), reads=[b_th], writes=[b_th])
    P.op("pool", lambda e: e.memset(zer, 0.0), writes=[b_th])
    CNT = A.f32(NSE); CNTb = A.bf16(NSE); b_cnt = Buf()
    M1f = M1.rearrange("p s n -> p (s n)"); M2f = M2.rearrange("p s n -> p (s n)")
    P.op("dve", lambda e: e.tensor_tensor(out=CNT, in0=M1f, in1=M2f, op=ALU.add), reads=[b_rt_info], writes=[b_cnt])
    P.op("dve", lambda e: e.tensor_copy(out=CNTb, in_=CNT), reads=[b_cnt], writes=[b_cnt])
    P.op("pe", lambda e: e.matmul(psum[0][:, 0:NSE], lhsT=triu, rhs=CNTb, start=True, stop=True), reads=[b_triu, b_cnt], writes=[pbuf[0]])
    P.op("pe", lambda e: e.matmul(psum[1][:, 0:NSE], lhsT=ones_bf, rhs=CNTb, start=True, stop=True), reads=[b_ones, b_cnt], writes=[pbuf[1]])
    TOTe = A.f32(NSE).rearrange("p (n s) -> p n s", n=NE); CUM = A.f32(NSE).rearrange("p (n s) -> p n s", n=NE)
    BO = A.f32(NSE).rearrange("p (n s) -> p n s", n=NE); b_rt2 = Buf()
    P.op("dve", lambda e: e.tensor_copy(out=TOTe, in_=psum[1][:, 0:NSE].rearrange("p (s n) -> p n s", n=NE)), reads=[pbuf[1]], writes=[b_rt2])
    for ex in range(NE):
        P.op("dve", lambda e, ex=ex: e.tensor_tensor_scan(out=CUM[:, ex, :], data0=TOTe[:, ex, :], data1=zer[:, 0:NS], initial=0.0, op0=ALU.add, op1=ALU.add),
             reads=[b_rt2, b_th], writes=[b_rt2])
    P.op("dve", lambda e: e.tensor_tensor(out=BO, in0=CUM, in1=TOTe, op=ALU.subtract), reads=[b_rt2], writes=[b_rt2])
    cmp8 = A.f32(NE * NJ).rearrange("p (n j) -> p n j", n=NE)
    PSz = A.f32(8); CUMPS = A.f32(8); OFF = A.f32(8)
    for ex in range(NE):
        P.op("dve", lambda e, ex=ex: e.tensor_scalar(out=cmp8[:, ex, :], in0=th8, scalar1=CUM[:, ex, NS - 1:NS], scalar2=None, op0=ALU.is_lt),
             reads=[b_rt2, b_th], writes=[b_rt2])
    P.op("dve", lambda e: e.tensor_reduce(out=PSz, in_=cmp8, axis=mybir.AxisListType.X, op=ALU.add), reads=[b_rt2], writes=[b_rt2])
    P.op("dve", lambda e: e.tensor_scalar(out=PSz, in0=PSz, scalar1=float(TS), scalar2=None, op0=ALU.mult), reads=[b_rt2], writes=[b_rt2])
    P.op("dve", lambda e: e.tensor_tensor_scan(out=CUMPS, data0=PSz, data1=zer[:, 0:8], initial=0.0, op0=ALU.add, op1=ALU.add), reads=[b_rt2, b_th], writes=[b_rt2])
    P.op("dve", lambda e: e.tensor_tensor(out=OFF, in0=CUMPS, in1=PSz, op=ALU.subtract), reads=[b_rt2], writes=[b_rt2])
    for ex in range(NE):
        P.op("dve", lambda e, ex=ex: e.tensor_scalar(out=BO[:, ex, :], in0=BO[:, ex, :], scalar1=OFF[:, ex:ex + 1], scalar2=None, op0=ALU.add),
             reads=[b_rt2], writes=[b_rt2])
    SLOT = A.f32(NSE).rearrange("p (s n) -> p s n", s=NS); STMP = A.f32(NSE).rearrange("p (s n) -> p s n", s=NS)
    P.op("dve", lambda e: e.tensor_tensor(out=SLOT, in0=psum[0][:, 0:NSE].rearrange("p (s n) -> p s n", n=NE), in1=BO.rearrange("p n s -> p s n"), op=ALU.add),
         reads=[pbuf[0], b_rt2], writes=[b_rt2])
    posf = A.f32(2 * NS).rearrange("p (k s) -> p k s", k=2); posi_w = A.f32(2 * NS); posi = posi_w.bitcast(I32).rearrange("p (k s) -> p k s", k=2); b_pos = Buf()
    for k_, Mk in enumerate((M1, M2)):
        P.op("dve", lambda e, Mk=Mk: e.tensor_tensor(out=STMP, in0=Mk, in1=SLOT, op=ALU.mult), reads=[b_rt_info, b_rt2], writes=[b_rt2])
        P.op("dve", lambda e, k_=k_: e.tensor_reduce(out=posf[:, k_, :], in_=STMP, axis=mybir.AxisListType.X, op=ALU.add), reads=[b_rt2], writes=[b_pos])
    P.op("dve", lambda e: e.tensor_copy(out=posi.rearrange("p k s -> p (k s)"), in_=posf.rearrange("p k s -> p (k s)")), reads=[b_pos], writes=[b_pos])
    cmpj = A.f32(NE * NSLOT_T).rearrange("p (n j) -> p n j", n=NE)
    EJf = A.f32(NSLOT_T + 1); EJi = A.f32(NSLOT_T + 1).bitcast(I32); b_ej = Buf()
    for ex in range(NE):
        P.op("dve", lambda e, ex=ex: e.tensor_scalar(out=cmpj[:, ex, :], in0=thj[:, 0:NSLOT_T], scalar1=CUMPS[:, ex:ex + 1], scalar2=None, op0=ALU.is_ge),
             reads=[b_rt2, b_th], writes=[b_rt2])
    P.op("dve", lambda e: e.tensor_reduce(out=EJf[:, 0:NSLOT_T], in_=cmpj.rearrange("p n j -> p j n"), axis=mybir.AxisListType.X, op=ALU.add), reads=[b_rt2], writes=[b_ej])
    P.op("dve", lambda e: e.tensor_scalar(out=EJf[:, 0:NSLOT_T], in0=EJf[:, 0:NSLOT_T], scalar1=float(NE - 1), scalar2=None, op0=ALU.min), reads=[b_ej], writes=[b_ej])
    P.op("dve", lambda e: e.tensor_copy(out=EJi[:, 0:NSLOT_T], in_=EJf[:, 0:NSLOT_T]), reads=[b_ej], writes=[b_ej])
    piota_i = A.f32(2).bitcast(I32); piota = A.f32(2); idxwf = A.f32(NSLOT_T + 1); idxw = A.f32(NSLOT_T + 1).bitcast(I32); b_idxw = Buf()
    P.op("pool", lambda e: e.iota(piota_i[:, 0:1], pattern=[[0, 1]], base=0, channel_multiplier=1), writes=[b_idxw])
    P.op("dve", lambda e: e.tensor_copy(out=piota[:, 0:1], in_=piota_i[:, 0:1]), reads=[b_idxw], writes=[b_idxw])
    P.op("dve", lambda e: e.tensor_scalar(out=idxwf[:, 0:NSLOT_T], in0=EJf[:, 0:NSLOT_T], scalar1=128.0, scalar2=piota[:, 0:1], op0=ALU.mult, op1=ALU.add),
         reads=[b_ej, b_idxw], writes=[b_idxw])
    P.op("dve", lambda e: e.tensor_copy(out=idxw[:, 0:NSLOT_T], in_=idxwf[:, 0:NSLOT_T]), reads=[b_idxw], writes=[b_idxw])
    if dbg:
        dump("d_posf", posf, [b_pos], F32); dump("d_ejf", EJf, [b_ej], F32); dump("d_M1", M1, [b_rt_info], F32); dump("d_M2", M2, [b_rt_info], F32)
        dump("d_G", G, [b_rt_info], F32)

    A.mark()
    xs_t = [A.bf16(D) for _ in range(8)]; b_xs = [Buf() for _ in range(8)]
    for si in range(NS):
        i3 = si % 8
        P.dma("sp", xs_t[i3], xnd[si * 128:(si + 1) * 128, :], reads=[b_xnd[si]], writes=[b_xs[i3]])
        for k_ in range(2):
            P.dma_custom("pool", lambda e, si=si, k_=k_, i3=i3: e.indirect_dma_start(
                out=Xg, out_offset=bass.IndirectOffsetOnAxis(ap=posi[:, k_, si:si + 1], axis=0), in_=xs_t[i3], in_offset=None),
                reads=[b_xs[i3], b_pos], writes=[b_Xg])
    P.barrier()
    A.release()

    A.mark()
    NCH = EXP // 128
    XgT = [A.bf16(KC * TS).rearrange("p (c t) -> p c t", c=KC) for _ in range(2)]; b_XgT = [Buf() for _ in range(2)]
    xgt = [A.bf16(D) for _ in range(4)]; b_xgt = [Buf() for _ in range(4)]
    hidT = A.bf16(NCH * TS).rearrange("p (c t) -> p c t", c=NCH); b_hid = [Buf() for _ in range(NCH)]
    QW = EXP // 4
    wgq_t = [A.bf16(KC * QW).rearrange("p (k n) -> p k n", k=KC) for _ in range(2)]; b_wgq = [Buf() for _ in range(2)]
    wuq_t = [A.bf16(KC * QW).rearrange("p (k n) -> p k n", k=KC) for _ in range(2)]; b_wuq = [Buf() for _ in range(2)]
    wdq_t = [A.bf16(7 * D).rearrange("p (c n) -> p c n", c=7) for _ in range(4)]; b_wdq = [Buf() for _ in range(4)]
    Yt = [A.f32(D) for _ in range(2)]; b_Yt = [Buf() for _ in range(2)]

    NSUB = TS // 128

    def load_xg_dma(j):
        for s in range(NSUB):
            q4 = (j * NSUB + s) % 4
            P.dma("sp", xgt[q4], Xg[j * TS + s * 128:j * TS + (s + 1) * 128, :], reads=[b_Xg], writes=[b_xgt[q4]])

    def load_xg_tr(j):
        xb_ = j % 2
        for s in range(NSUB):
            q4 = (j * NSUB + s) % 4
            pb = psum[7].bitcast(BF16)
            for c in range(KC):
                P.op("pe", lambda e, c=c, q4=q4, pb=pb: e.transpose(out=pb[:, c * 128:(c + 1) * 128], in_=xgt[q4][:, c * 128:(c + 1) * 128], identity=ident),
                     reads=[b_xgt[q4], b_ident], writes=[pbuf[7]])
            P.op("act", lambda e, s=s, xb_=xb_, pb=pb: e.activation(out=XgT[xb_][:, :, s * 128:(s + 1) * 128], in_=pb.rearrange("p (c t) -> p c t", c=KC), func=AF.Copy),
                 reads=[pbuf[7]], writes=[b_XgT[xb_]])

    load_xg_dma(0)
    load_xg_tr(0)
    for j in range(NSLOT_T):
        xb_ = j % 2

        def moe_gu(g, sl, j=j):
            P.dma_custom("pool", lambda e, g=g, sl=sl, j=j: e.indirect_dma_start(
                out=wgq_t[sl].rearrange("p k n -> p (k n)"), out_offset=None, in_=moe_gq[g],
                in_offset=bass.IndirectOffsetOnAxis(ap=idxw[:, j:j + 1], axis=0)), reads=[b_idxw] + b_moe, writes=[b_wgq[sl]])
            P.dma_custom("pool", lambda e, g=g, sl=sl, j=j: e.indirect_dma_start(
                out=wuq_t[sl].rearrange("p k n -> p (k n)"), out_offset=None, in_=moe_uq[g],
                in_offset=bass.IndirectOffsetOnAxis(ap=idxw[:, j:j + 1], axis=0)), reads=[b_idxw] + b_moe, writes=[b_wuq[sl]])

        def moe_wd(g, j=j):
            P.dma_custom("pool", lambda e, g=g, j=j: e.indirect_dma_start(
                out=wdq_t[g].rearrange("p c n -> p (c n)"), out_offset=None, in_=moe_dq[g],
                in_offset=bass.IndirectOffsetOnAxis(ap=idxw[:, j:j + 1], axis=0)), reads=[b_idxw] + b_moe, writes=[b_wdq[g]])

        def sink(s, d, pd, j=j):
            yb = s % 2
            if d == 0:
                P.op("act", lambda e: e.activation(out=Yt[yb][:, d * 512:(d + 1) * 512], in_=psum[pd], func=AF.Copy), reads=[pbuf[pd]], writes=[b_Yt[yb]])
            else:
                P.op("dve", lambda e: e.tensor_copy(out=Yt[yb][:, d * 512:(d + 1) * 512], in_=psum[pd]), reads=[pbuf[pd]], writes=[b_Yt[yb]])
                P.dma("sp", Yd[j * TS + s * 128:j * TS + (s + 1) * 128, :], Yt[yb], reads=[b_Yt[yb]], writes=[b_Yd])

        hk = [0]

        def hook(j=j, hk=hk):
            hk[0] += 1
            if j + 1 < NSLOT_T:
                if hk[0] == 2:
                    load_xg_dma(j + 1)
                elif hk[0] == 12:
                    load_xg_tr(j + 1)

        swiglu_core(NCH, 7, wgq_t, wuq_t, b_wgq, b_wuq, moe_gu, moe_wd, 4, XgT[xb_], b_XgT[xb_], hidT, b_hid,
                    lambda c: (wdq_t[c // 7][:, c % 7, :], b_wdq[c // 7]), sink, ntok=TS, hook=hook)
    P.barrier()
    A.release()

    A.mark()
    NB5 = 6
    Y1 = [A.f32(D) for _ in range(NB5)]; Y2 = [A.f32(D) for _ in range(NB5)]; Rt = [A.f32(D) for _ in range(NB5)]
    b_Y1 = [Buf() for _ in range(NB5)]; b_Y2 = [Buf() for _ in range(NB5)]; b_Rt = [Buf() for _ in range(NB5)]
    ot = [A.f32(D) for _ in range(NB5)]; b_ot = [Buf() for _ in range(NB5)]
    ntmp = norm_tmp()
    g4_bc, b_g4 = load_bcast(final_g, D, "g4")
    junk, ss, rstd, xn_unused, b_tmp = ntmp
    for si in range(NS):
        i2 = si % NB5
        P.dma_custom("pool", lambda e, si=si, i2=i2: e.indirect_dma_start(
            out=Y1[i2], out_offset=None, in_=Yd, in_offset=bass.IndirectOffsetOnAxis(ap=posi[:, 0, si:si + 1], axis=0)),
            reads=[b_Yd, b_pos], writes=[b_Y1[i2]])
        P.dma_custom("pool", lambda e, si=si, i2=i2: e.indirect_dma_start(
            out=Y2[i2], out_offset=None, in_=Yd, in_offset=bass.IndirectOffsetOnAxis(ap=posi[:, 1, si:si + 1], axis=0)),
            reads=[b_Yd, b_pos], writes=[b_Y2[i2]])
        P.dma("sp", Rt[i2], h3d[si * 128:(si + 1) * 128, :], reads=[b_h3[si]], writes=[b_Rt[i2]])
        P.op("dve", lambda e, si=si, i2=i2: e.scalar_tensor_tensor(out=Rt[i2], in0=Y1[i2], scalar=G[:, si, 0:1], in1=Rt[i2], op0=ALU.mult, op1=ALU.add),
             reads=[b_Y1[i2], b_Rt[i2], b_rt_info], writes=[b_Rt[i2]])
        P.op("dve", lambda e, si=si, i2=i2: e.scalar_tensor_tensor(out=Rt[i2], in0=Y2[i2], scalar=G[:, si, 1:2], in1=Rt[i2], op0=ALU.mult, op1=ALU.add),
             reads=[b_Y2[i2], b_Rt[i2], b_rt_info], writes=[b_Rt[i2]])
        P.op("act", lambda e, i2=i2: e.activation(out=junk, in_=Rt[i2], func=AF.Square, accum_out=ss), reads=[b_Rt[i2]], writes=[b_tmp[0]])
        P.op("act", lambda e: e.activation(out=rstd, in_=ss, func=AF.Ln, scale=1.0 / D, bias=cst[:, 1:2]), reads=[b_tmp[0], b_cst], writes=[b_tmp[1]])
        P.op("act", lambda e: e.activation(out=rstd, in_=rstd, func=AF.Exp, scale=-0.5), reads=[b_tmp[1]], writes=[b_tmp[1]])
        P.op("dve", lambda e, i2=i2: e.scalar_tensor_tensor(out=ot[i2], in0=Rt[i2], scalar=rstd, in1=g4_bc, op0=ALU.mult, op1=ALU.mult),
             reads=[b_Rt[i2], b_tmp[1], b_g4], writes=[b_ot[i2]])
        P.dma("sp", y[si * 128:(si + 1) * 128, :], ot[i2], reads=[b_ot[i2]], writes=[ybuf])
    P.final_wait("sp")
    P.emit()
    return nc


def _consts(S):
    half = 8
    inv = 500000.0 ** (-np.arange(0, 16, 2, dtype=np.float32) / 16.0)
    ang = np.arange(S, dtype=np.float32)[:, None] * inv[None, :]
    cos = np.cos(ang).astype(np.float32).T
    sin = np.sin(ang).astype(np.float32).T
    C = np.ones((128, S), np.float32); Sg = np.zeros((128, S), np.float32)
    for m in range(2):
        b = m * 64
        C[b:b + 8] = cos; C[b + 8:b + 16] = cos
        Sg[b:b + 8] = -sin; Sg[b + 8:b + 16] = sin
    ropet = np.stack([C * 0.125, Sg * 0.125, C, Sg]).astype(np.float32)
    invcnt = np.zeros((128, 8, 16), np.float32)
    for g, w in enumerate((2, 4, 8, 16)):
        v = 1.0 / np.minimum(np.arange(16) + 1, w).astype(np.float32)
        invcnt[:, 2 * g] = v; invcnt[:, 2 * g + 1] = v
    return ropet, invcnt


_NC_CACHE = {}


def make_in_maps(inputs, S, nb):
    ropet, invcnt = _consts(S)
    sq = lambda a: np.ascontiguousarray(np.asarray(a)[0])
    shared = {
        "ln_mix_even": sq(inputs["ln_mix_even"]), "ln_ffn_even": sq(inputs["ln_ffn_even"]),
        "ln_mix_odd": sq(inputs["ln_mix_odd"]), "ln_ffn_odd": sq(inputs["ln_ffn_odd"]),
        "final_g": np.ascontiguousarray(np.asarray(inputs["final_g"])), "pool_scale": sq(inputs["pool_scale"]),
        "w_in_even": sq(inputs["w_in_even"]), "w_out_even": sq(inputs["w_out_even"]),
        "lam_q1": sq(inputs["lam_q1"]), "lam_k1": sq(inputs["lam_k1"]), "lam_q2": sq(inputs["lam_q2"]), "lam_k2": sq(inputs["lam_k2"]),
        "subln_g": sq(inputs["subln_g"]), "conv_w": sq(inputs["conv_w"]), "conv_b": sq(inputs["conv_b"]),
        "rg_wa": sq(inputs["rg_wa"]), "rg_ba": sq(inputs["rg_ba"]).reshape(512),
        "rg_wx": sq(inputs["rg_wx"]), "rg_bx": sq(inputs["rg_bx"]).reshape(512), "rg_lam": sq(inputs["rg_lam"]),
        "ffn_wg": sq(inputs["ffn_wg"]), "ffn_wu": sq(inputs["ffn_wu"]), "ffn_wd": sq(inputs["ffn_wd"]),
        "pool_w": sq(inputs["pool_w"]), "router_w": sq(inputs["router_w"]),
        "moe_wg": sq(inputs["moe_wg"]), "moe_wu": sq(inputs["moe_wu"]), "moe_wd": sq(inputs["moe_wd"]),
        "ropet": ropet, "invcnt": invcnt,
    }
    xs = np.asarray(inputs["x"])
    maps = []
    for b in range(nb):
        m = dict(shared)
        m["x"] = np.ascontiguousarray(xs[b])
        maps.append(m)
    return maps


def kernel(**inputs):
    x = np.asarray(inputs["x"])
    B, S, _ = x.shape
    if S not in _NC_CACHE:
        _NC_CACHE[S] = build(S)
    nc = _NC_CACHE[S]
    in_maps = make_in_maps(inputs, S, B)
    res = run_bass_kernel_spmd(nc, in_maps, core_ids=list(range(B)))
    return np.stack([np.asarray(r["y"]) for r in res.results], axis=0).astype(np.float32)
```
